# Optimizing a Trainium2 kernel written in Bass

```python
import jax, jax.numpy as jnp
from jax import lax
import numpy as np

D_MODEL = 1024
BATCH = 4
SEQ = 8192
DEPTH = 1

D_CONV = D_MODEL
CONV_WIDTH = 3
CONV_GROUPS = 8
D_POOL = D_MODEL
POOL_WINDOWS = (2, 4, 8, 16)
N_POOL_GROUPS = len(POOL_WINDOWS)
POOL_GROUP = D_POOL // N_POOL_GROUPS
IN_COLS = 3 * D_CONV + D_POOL + 2 * D_MODEL
IN_SPLITS = (D_CONV, 2 * D_CONV, 3 * D_CONV, 3 * D_CONV + D_POOL, 3 * D_CONV + D_POOL + D_MODEL)
N_GROUPS = 8
EXPERTS_PER_GROUP = 8
N_EXPERTS = N_GROUPS * EXPERTS_PER_GROUP
TOP_K = 2
D_EXPERT = 512
MOE_BLOCK = 128
LN_EPS = 1e-5
DEEPNORM_ALPHA = (2.0 * DEPTH) ** 0.25
DEEPNORM_BETA = (8.0 * DEPTH) ** -0.25
N_MOD = 6

kernel_name = "hybrid_conv_pool_hmoe_deepnorm_adaln"


def layer_norm(x, g, b):
    xf = x.astype(jnp.float32)
    mu = jnp.mean(xf, axis=-1, keepdims=True)
    var = jnp.mean(jnp.square(xf - mu), axis=-1, keepdims=True)
    y = (xf - mu) * lax.rsqrt(var + LN_EPS) * g.astype(jnp.float32) + b.astype(jnp.float32)
    return y.astype(x.dtype)


def causal_depthwise_conv(z, w):
    return lax.conv_general_dilated(
        z, w[:, None, :].astype(z.dtype), window_strides=(1,),
        padding=((CONV_WIDTH - 1, 0),), dimension_numbers=('NWC', 'WIO', 'NWC'),
        feature_group_count=z.shape[-1])


def causal_multiscale_pool(z):
    S = z.shape[1]
    zf = z.astype(jnp.float32)
    cs = jnp.pad(jnp.cumsum(zf, axis=1), ((0, 0), (1, 0), (0, 0)))
    t = jnp.arange(S)
    outs = []
    for gi, w in enumerate(POOL_WINDOWS):
        c = cs[:, :, gi * POOL_GROUP:(gi + 1) * POOL_GROUP]
        lower = jnp.pad(c[:, :S + 1 - w], ((0, 0), (w - 1, 0), (0, 0)))
        count = jnp.minimum(t + 1, w).astype(jnp.float32)[None, :, None]
        outs.append((c[:, 1:] - lower) / count - zf[:, :, gi * POOL_GROUP:(gi + 1) * POOL_GROUP])
    return jnp.stack(outs, axis=2).astype(z.dtype)


def hybrid_mixer(u, w_in, conv_w, w_out_conv, w_pool, pool_scale, w_o):
    proj = jnp.einsum('bsd,de->bse', u, w_in)
    a_val, a_b, a_c, p_in, g_a, g_b = jnp.split(proj, IN_SPLITS, axis=-1)
    y_a = causal_depthwise_conv(a_c * a_val, conv_w) * a_b
    y_a = jnp.einsum('bsc,cd->bsd', y_a, w_out_conv)
    pooled = causal_multiscale_pool(p_in)
    y_b = jnp.einsum('bsgc,gce->bsge', pooled, w_pool)
    y_b = y_b.reshape(u.shape[0], u.shape[1], D_POOL) * pool_scale
    merged = jax.nn.sigmoid(g_a) * y_a + jax.nn.sigmoid(g_b) * y_b
    return jnp.einsum('bsd,de->bse', merged, w_o)


def hierarchical_route(h, w_group, b_group, w_router, b_router):
    n = h.shape[0]
    hf = h.astype(jnp.float32)
    g_prob = jax.nn.softmax(hf @ w_group.astype(jnp.float32) + b_group.astype(jnp.float32), axis=-1)
    g_top_p, g_top = lax.top_k(g_prob, 1)
    e_logits = (hf @ w_router.astype(jnp.float32)).reshape(n, N_GROUPS, EXPERTS_PER_GROUP)
    e_logits = e_logits + b_router.astype(jnp.float32)
    e_logits = jnp.take_along_axis(e_logits, g_top[:, :, None], axis=1)[:, 0]
    e_prob = jax.nn.softmax(e_logits, axis=-1)
    e_top_p, e_top = lax.top_k(e_prob, TOP_K)
    e_top_p = e_top_p / jnp.sum(e_top_p, axis=-1, keepdims=True)
    return g_top * EXPERTS_PER_GROUP + e_top, g_top_p * e_top_p


def routed_experts(h, expert_idx, expert_w, w_gate, w_up, w_down):
    n = h.shape[0]
    n_assign = n * TOP_K
    n_blocks = -(-n_assign // MOE_BLOCK) + N_EXPERTS
    cap = n_blocks * MOE_BLOCK
    flat_e = expert_idx.reshape(-1).astype(jnp.int32)
    order = jnp.argsort(flat_e)
    sorted_e = flat_e[order]
    token_of = (order // TOP_K).astype(jnp.int32)
    counts = jnp.bincount(flat_e, length=N_EXPERTS)
    padded = (counts + MOE_BLOCK - 1) // MOE_BLOCK * MOE_BLOCK
    pad_end = jnp.cumsum(padded)
    pad_start = pad_end - padded
    seg_start = jnp.cumsum(counts) - counts
    dest = pad_start[sorted_e] + jnp.arange(n_assign, dtype=jnp.int32) - seg_start[sorted_e]
    buf_tok = jnp.zeros((cap,), jnp.int32).at[dest].set(token_of)
    buf_w = jnp.zeros((cap,), h.dtype).at[dest].set(expert_w.reshape(-1)[order].astype(h.dtype))
    block_expert = jnp.minimum(
        jnp.searchsorted(pad_end, jnp.arange(n_blocks) * MOE_BLOCK, side='right'), N_EXPERTS - 1)

    def expert_block(args):
        tok, e = args
        xb = h[tok]
        act = jax.nn.silu(xb @ w_gate[e]) * (xb @ w_up[e])
        return act @ w_down[e]

    rows = lax.map(expert_block, (buf_tok.reshape(n_blocks, MOE_BLOCK), block_expert))
    rows = rows.reshape(cap, h.shape[1]) * buf_w[:, None]
    return jax.ops.segment_sum(rows, buf_tok, num_segments=n)


def setup_inputs(seed: int = 0) -> dict:
    key = jax.random.key(seed)
    ks = jax.random.split(key, 24)
    L = DEPTH
    beta = DEEPNORM_BETA

    def nrm(k, shape, s):
        return jax.random.normal(k, shape, jnp.float32) * s

    col_scale = jnp.concatenate([
        jnp.full((D_CONV,), beta, jnp.float32), jnp.ones((2 * D_CONV,), jnp.float32),
        jnp.full((D_POOL,), beta, jnp.float32), jnp.ones((2 * D_MODEL,), jnp.float32)])
    return {
        "x": nrm(ks[0], (BATCH, SEQ, D_MODEL), 1.0),
        "c": nrm(ks[1], (BATCH, D_MODEL), 1.0),
        "w_ada": nrm(ks[2], (L, D_MODEL, N_MOD * D_MODEL), 0.1 * D_MODEL ** -0.5),
        "b_ada": nrm(ks[3], (L, N_MOD * D_MODEL), 0.01),
        "w_in": nrm(ks[4], (L, D_MODEL, IN_COLS), D_MODEL ** -0.5) * col_scale,
        "conv_w": nrm(ks[5], (L, CONV_WIDTH, D_CONV), CONV_WIDTH ** -0.5),
        "w_out_conv": nrm(ks[6], (L, D_CONV, D_MODEL), beta * D_CONV ** -0.5),
        "w_pool": nrm(ks[7], (L, N_POOL_GROUPS, POOL_GROUP, POOL_GROUP), beta * POOL_GROUP ** -0.5),
        "pool_scale": 1.0 + nrm(ks[8], (L, D_POOL), 0.1),
        "w_o": nrm(ks[9], (L, D_MODEL, D_MODEL), beta * D_MODEL ** -0.5),
        "ln1_g": 1.0 + nrm(ks[10], (L, D_MODEL), 0.05),
        "ln1_b": nrm(ks[11], (L, D_MODEL), 0.02),
        "w_group": nrm(ks[12], (L, D_MODEL, N_GROUPS), D_MODEL ** -0.5),
        "b_group": nrm(ks[13], (L, N_GROUPS), 0.01),
        "w_router": nrm(ks[14], (L, D_MODEL, N_EXPERTS), D_MODEL ** -0.5),
        "b_router": nrm(ks[15], (L, N_GROUPS, EXPERTS_PER_GROUP), 0.01),
        "w_gate": nrm(ks[16], (L, N_EXPERTS, D_MODEL, D_EXPERT), D_MODEL ** -0.5),
        "w_up": nrm(ks[17], (L, N_EXPERTS, D_MODEL, D_EXPERT), beta * D_MODEL ** -0.5),
        "w_down": nrm(ks[18], (L, N_EXPERTS, D_EXPERT, D_MODEL), beta * D_EXPERT ** -0.5),
        "ln2_g": 1.0 + nrm(ks[19], (L, D_MODEL), 0.05),
        "ln2_b": nrm(ks[20], (L, D_MODEL), 0.02),
    }


def reference(x, c, w_ada, b_ada, w_in, conv_w, w_out_conv, w_pool, pool_scale, w_o,
              ln1_g, ln1_b, w_group, b_group, w_router, b_router, w_gate, w_up, w_down,
              ln2_g, ln2_b):
    bsz, seq, d = x.shape
    for l in range(DEPTH):
        mod = (jax.nn.silu(c) @ w_ada[l] + b_ada[l]).reshape(bsz, N_MOD, d)[:, :, None, :]
        shift1, scale1, gate1, shift2, scale2, gate2 = [mod[:, i] for i in range(N_MOD)]
        u = x * (1.0 + scale1) + shift1
        mix = hybrid_mixer(u, w_in[l], conv_w[l], w_out_conv[l], w_pool[l], pool_scale[l], w_o[l])
        x = layer_norm(DEEPNORM_ALPHA * x + (1.0 + gate1) * mix, ln1_g[l], ln1_b[l])
        h = (x * (1.0 + scale2) + shift2).reshape(bsz * seq, d)
        idx, wts = hierarchical_route(h, w_group[l], b_group[l], w_router[l], b_router[l])
        y = routed_experts(h, idx, wts, w_gate[l], w_up[l], w_down[l]).reshape(bsz, seq, d)
        x = layer_norm(DEEPNORM_ALPHA * x + (1.0 + gate2) * y, ln2_g[l], ln2_b[l])
    return x
```

```python
import contextlib
import numpy as np
import concourse.bass as bass
import concourse.mybir as mybir
from concourse.bass_utils import run_bass_kernel_spmd

F32 = mybir.dt.float32
BF16 = mybir.dt.bfloat16
I32 = mybir.dt.int32
AF = mybir.ActivationFunctionType
ALU = mybir.AluOpType
AX = mybir.AxisListType

NCORES = 8
D = 1024
KD = 8
SEQ = 8192
TOK = 4096
HL = 16
T = 256
NT = TOK // T
NSUB = TOK // 128
NEXP = 64
DEXP = 512
CAP = 256
NBLK = NEXP * CAP // 128
WINS = (2, 4, 8, 16)
ALPHA = 2.0 ** 0.25
EPS = 1e-5
NCOLS = 6144
NPRE = 48

ENGS = ("pe", "act", "dve", "pool", "sp")


class _Op:
    __slots__ = ("eng", "fn", "deps", "key", "signal", "idx", "dmaval")

    def __init__(self, eng, fn, key):
        self.eng = eng
        self.fn = fn
        self.key = key
        self.deps = []
        self.signal = False
        self.idx = 0
        self.dmaval = 0


class Sched:
    def __init__(self):
        self.ops = {e: [] for e in ENGS}
        self.last_w = {}
        self.readers = {}
        self.dma_cnt = {}
        self.bar = {}
        self.n = 0

    def op(self, eng, fn, r=(), w=(), key=None):
        o = _Op(eng, fn, key)
        deps = {}
        if eng in self.bar:
            for d in self.bar.pop(eng):
                deps[id(d[1]) if d[0] == "op" else d[1]] = d
        for res in r:
            lw = self.last_w.get(res)
            if lw is not None:
                deps[id(lw)] = ("op", lw)
            if res[0] == "ps":
                for rd in self.readers.get(res, ()):
                    if rd.eng != eng:
                        deps[id(rd)] = ("op", rd)
        for res in w:
            lw = self.last_w.get(res)
            if lw is not None:
                deps[id(lw)] = ("op", lw)
            for rd in self.readers.get(res, ()):
                deps[id(rd)] = ("op", rd)
        has_reader_dep = any(d[0] == "op" and d[1].key is None for d in deps.values())
        for d in deps.values():
            if d[0] == "dma":
                o.deps.append(d)
                continue
            p = d[1]
            if p is o:
                continue
            if p.key is not None:
                if key == p.key and has_reader_dep:
                    continue
                o.deps.append(("dma", p.key, self.dma_cnt[p.key] * 16))
            else:
                if eng == "pe" and p.eng == "pe":
                    continue
                p.signal = True
                o.deps.append(("op", p))
        if key is not None:
            self.dma_cnt[key] = self.dma_cnt.get(key, 0) + 1
            o.dmaval = self.dma_cnt[key] * 16
        for res in w:
            self.last_w[res] = o
            self.readers[res] = []
        for res in r:
            self.readers.setdefault(res, []).append(o)
        self.ops[eng].append(o)
        self.n += 1
        return o

    def barrier(self):
        deps = []
        for e in ("pe", "act", "dve", "pool"):
            if self.ops[e]:
                for o in reversed(self.ops[e]):
                    if o.key is None:
                        o.signal = True
                        deps.append(("op", o))
                        break
        for k, c in self.dma_cnt.items():
            deps.append(("dma", k, c * 16))
        for e in ENGS:
            self.bar[e] = list(deps)
        self.last_w = {}
        self.readers = {}

    def number(self):
        for e in ENGS:
            i = 0
            for o in self.ops[e]:
                if o.key is None and o.signal:
                    i += 1
                    o.idx = i

    def emit_engine(self, e, eng, engsem, dmasem):
        waited = {}
        for o in self.ops[e]:
            need = {}
            for d in o.deps:
                if d[0] == "dma":
                    sem, val = dmasem[d[1]], d[2]
                else:
                    sem, val = engsem[d[1].eng], d[1].idx
                if need.get(id(sem), (None, 0))[1] < val:
                    need[id(sem)] = (sem, val)
            for sem, val in need.values():
                if waited.get(id(sem), 0) < val:
                    eng.wait_ge(sem, val)
                    waited[id(sem)] = val
            ins = o.fn(eng)
            if o.key is not None:
                ins.then_inc(dmasem[o.key], 16)
            elif o.signal:
                ins.then_inc(engsem[e], 1)
        if e == "sp":
            for k, c in self.dma_cnt.items():
                eng.wait_ge(dmasem[k], c * 16)


def build_program(debug=False):
    nc = bass.Bass("TRN2", target_bir_lowering=False)

    def din(name, shape, dt=F32):
        return nc.dram_tensor(name, list(shape), dt, kind="ExternalInput").ap()

    xs = din("xs", [HL + TOK, D])
    cvec = din("cvec", [128, KD])
    hm = din("hm", [128, 1])
    icnt = din("icnt", [128, 4 * T])
    w_ada = din("w_ada", [D, NCOLS])
    b_adaT = din("b_adaT", [128, 48])
    w_in = din("w_in", [D, NCOLS])
    cwT = din("cwT", [128, KD * 3])
    w_oc = din("w_oc", [D, D])
    w_pool = din("w_pool", [4, 256, 256])
    pscT = din("pscT", [128, KD])
    w_o = din("w_o", [D, D])
    ln1gT = din("ln1gT", [128, KD])
    ln1bT = din("ln1bT", [128, KD])
    ln1g = din("ln1g", [D])
    ln1b = din("ln1b", [D])
    ln2g = din("ln2g", [D])
    ln2b = din("ln2b", [D])
    w_rt = din("w_rt", [D, 72])
    b_rt = din("b_rt", [72])
    w_gate = din("w_gate", [NEXP, D, DEXP])
    w_up = din("w_up", [NEXP, D, DEXP])
    w_down = din("w_down", [NEXP, DEXP, D])
    out = nc.dram_tensor("out", [TOK, D], F32, kind="ExternalOutput").ap()
    skind = "ExternalOutput" if debug else "Internal"
    n_dram = nc.dram_tensor("n_dram", [TOK, D], F32, kind=skind).ap()
    rows_dram = nc.dram_tensor("rows_dram", [NEXP * CAP, D], F32, kind="Internal").ap()
    tokbuf = nc.dram_tensor("tokbuf", [NEXP * CAP, 1], F32, kind=skind).ap()
    wgb = nc.dram_tensor("wgb", [NPRE, D, DEXP], BF16, kind="Internal").ap()
    wub = nc.dram_tensor("wub", [NPRE, D, DEXP], BF16, kind="Internal").ap()
    wdb = nc.dram_tensor("wdb", [NPRE, DEXP, D], BF16, kind="Internal").ap()
    if debug:
        dbg = nc.dram_tensor("dbg", [128, 4 * NSUB], F32, kind="ExternalOutput").ap()

    S = Sched()
    AW = 53150
    with (
        nc.sbuf_tensor("arena", [128, AW], F32) as arena,
        nc.psum_tensor("ps0", [128, 512], F32) as ps0,
        nc.psum_tensor("ps1", [128, 512], F32) as ps1,
        nc.psum_tensor("ps2", [128, 512], F32) as ps2,
        nc.psum_tensor("ps3", [128, 512], F32) as ps3,
        nc.psum_tensor("ps4", [128, 512], F32) as ps4,
        nc.psum_tensor("ps5", [128, 512], F32) as ps5,
        nc.psum_tensor("ps6", [128, 512], F32) as ps6,
        nc.psum_tensor("ps7", [128, 512], F32) as ps7,
    ):
        psb = [ps0, ps1, ps2, ps3, ps4, ps5, ps6, ps7]
        st = {"off": 0, "uid": 0}

        def alloc(n, dt=F32):
            words = n if dt in (F32, I32) else (n + 1) // 2
            words = (words + 1) // 2 * 2
            a = arena[:, st["off"]:st["off"] + words]
            st["off"] += words
            assert st["off"] <= AW, st["off"]
            st["uid"] += 1
            if dt != F32:
                a = a.bitcast(dt)
            a = a[:, 0:n]
            return a, ("sb", st["uid"])

        def alloc_pair(n):
            n2 = (n + 1) // 2 * 2
            off = st["off"]
            a, ra = alloc(n)
            b, rb = alloc(n)
            pv = arena[:, off:off + 2 * n2].rearrange("p (c n) -> p c n", c=2)[:, :, 0:n]
            return pv, [(a, ra), (b, rb)]

        pst = {"i": 0, "nb": 7}

        def half():
            n2 = pst["nb"] * 2
            i = pst["i"] % n2
            pst["i"] += 1
            b, hh = i // 2, i % 2
            return psb[b][:, hh * 256:(hh + 1) * 256], ("ps", b)

        def pair():
            p, r = full()
            return p.rearrange("p (c n) -> p c n", c=2), r

        def full():
            if pst["i"] % 2:
                pst["i"] += 1
            n2 = pst["nb"] * 2
            b = (pst["i"] % n2) // 2
            pst["i"] += 2
            return psb[b][:, :], ("ps", b)

        ident, r_ident = alloc(128)
        ustr, r_ustr = alloc(128)
        ones, r_ones = alloc(128)
        eC, r_eC = alloc(64)
        eCi, r_eCi = alloc(64, I32)
        tokidi, r_tokidi = alloc(NSUB, I32)
        tokidf, r_tokidf = alloc(NSUB)
        epst, r_eps = alloc(1)
        cvt, r_cvt = alloc(KD)
        cst, r_cs = alloc(KD, BF16)
        modT, r_modT = alloc(48)
        badT, r_badT = alloc(48)
        sh1, r_sh1 = alloc(KD)
        sc1p, r_sc1p = alloc(KD)
        A2, r_A2 = alloc(KD)
        B2, r_B2 = alloc(KD)
        g1p, r_g1p = alloc(KD)
        g2p, r_g2p = alloc(KD)
        l1gT, r_l1gT = alloc(KD)
        l1bT, r_l1bT = alloc(KD)
        cw, r_cw = alloc(KD * 3)
        psc, r_psc = alloc(KD)
        hmt, r_hm = alloc(1)
        brt, r_brt = alloc(72)
        S1f, r_S1f = alloc(NSUB)
        S2f, r_S2f = alloc(NSUB)
        W1, r_W1 = alloc(NSUB)
        W2, r_W2 = alloc(NSUB)
        Acc, r_Acc = alloc(64)
        persist_end = st["off"]

        S.op("pool", lambda g: g.memset(ident, 1.0), w=[r_ident])
        S.op("pool", lambda g: g.affine_select(out=ident, in_=ident, pattern=[[-1, 128]], compare_op=ALU.is_equal,
                                               fill=0.0, base=0, channel_multiplier=1), r=[r_ident], w=[r_ident])
        S.op("pool", lambda g: g.memset(ustr, 1.0), w=[r_ustr])
        S.op("pool", lambda g: g.affine_select(out=ustr, in_=ustr, pattern=[[1, 128]], compare_op=ALU.is_gt,
                                               fill=0.0, base=0, channel_multiplier=-1), r=[r_ustr], w=[r_ustr])
        S.op("pool", lambda g: g.memset(ones, 1.0), w=[r_ones])
        S.op("pool", lambda g: g.memset(epst, EPS), w=[r_eps])
        S.op("pool", lambda g: g.memset(Acc, 0.0), w=[r_Acc])
        S.op("pool", lambda g: g.iota(eCi, pattern=[[CAP, 64]], base=0, channel_multiplier=0), w=[r_eCi])
        S.op("pool", lambda g: g.tensor_copy(out=eC, in_=eCi), r=[r_eCi], w=[r_eC])
        S.op("pool", lambda g: g.iota(tokidi, pattern=[[128, NSUB]], base=0, channel_multiplier=1), w=[r_tokidi])
        S.op("pool", lambda g: g.tensor_copy(out=tokidf, in_=tokidi), r=[r_tokidi], w=[r_tokidf])

        def ld(dst, rdst, src, key):
            S.op("sp", lambda q: q.dma_start(out=dst, in_=src), w=[rdst], key=key)

        ld(cvt, r_cvt, cvec, "k_cvt")
        ld(badT, r_badT, b_adaT, "k_bad")
        ld(l1gT, r_l1gT, ln1gT, "k_l1g")
        ld(l1bT, r_l1bT, ln1bT, "k_l1b")
        ld(cw, r_cw, cwT, "k_cw")
        ld(psc, r_psc, pscT, "k_psc")
        ld(hmt, r_hm, hm, "k_hm")
        ld(brt, r_brt, b_rt.partition_broadcast(128), "k_brt")

        winb, _ = alloc(KD * NCOLS, BF16)
        winv = winb.rearrange("p (k n) -> p k n", k=KD)
        wocb, r_woc = alloc(KD * D, BF16)
        wocv = wocb.rearrange("p (k n) -> p k n", k=KD)
        wob, r_wo = alloc(KD * D, BF16)
        wov = wob.rearrange("p (k n) -> p k n", k=KD)
        wa = [(wocv, r_woc), (wov, r_wo)]
        wplb, r_wpl = alloc(4 * 2 * 256, BF16)
        wplv = wplb.rearrange("p (g k n) -> p g k n", g=4, k=2)
        wrt, r_wrt = alloc(KD * 72)
        wrtv = wrt.rearrange("p (k n) -> p k n", k=KD)
        bc_g1, r_bcg1 = alloc(D)
        xin = [alloc(D), alloc(D)]
        xres = [alloc(D), alloc(D)]
        uT, _r = alloc(KD * T, BF16)
        r_uTs = [("uT", i_) for i_ in range(4)]
        uTv = uT.rearrange("p (k n) -> p k n", k=KD)
        ztp, zt = alloc_pair(T + 2)
        zcar, r_zcar = alloc(KD * 2)
        avp, avalb = alloc_pair(T)
        t1p, ct1 = alloc_pair(T)
        t2p, ct2 = alloc_pair(T)
        ct = ct1 + ct2
        pbp, pbt = alloc_pair(T + 15)
        pcar, r_pcar = alloc(KD * 15)
        ptmA, ptm01 = alloc_pair(T + 15)
        ptmB, ptm23 = alloc_pair(T + 15)
        ptm = ptm01 + ptm23
        stt, r_stt = alloc(12)
        mvt, r_mvt = alloc(2)
        rstd, r_rstd = alloc(1)
        nbm, r_nbm = alloc(1)
        tmpv = [alloc(512), alloc(512)]
        ict = arena[:, st["off"] - 1024:st["off"]]
        r_ict = [tmpv[0][1], tmpv[1][1]]
        dgs = [(ct1[0][0][:, 0:128], ct1[0][1]), (ct1[1][0][:, 0:128], ct1[1][1])]
        yaT, _r = alloc(KD * T, BF16)
        r_yaTs = [("yaT", i_) for i_ in range(4)]
        yaTv = yaT.rearrange("p (k n) -> p k n", k=KD)
        poT, _r = alloc(KD * T, BF16)
        r_poTs = [("poT", i_) for i_ in range(4)]
        poTv = poT.rearrange("p (k n) -> p k n", k=KD)
        meT, _r = alloc(KD * T, BF16)
        r_meTs = [("meT", i_) for i_ in range(8)]
        meTv = meT.rearrange("p (k n) -> p k n", k=KD)
        sga = [alloc(T), alloc(T)]
        sgb = [alloc(T), alloc(T)]
        m1b = [alloc(T), alloc(T)]
        m2b = [alloc(T)] * 2
        hT, r_hT = alloc(KD * 128)
        r_hTs = [("hT", 0), ("hT", 1)]
        hTv = hT.rearrange("p (k n) -> p k n", k=KD)
        lg, r_lg = alloc(72)
        mx8, r_mx8 = alloc(8)
        gsel, r_gsel = alloc(8)
        ngm, r_ngm = alloc(1)
        ex8, r_ex8 = alloc(8)
        sme, r_sme = alloc(1)
        gp, r_gp = alloc(1)
        t64, r_t64 = alloc(64)
        t64b, r_t64b = alloc(64)
        el, r_el = alloc(8)
        em8, r_em8 = alloc(8)
        oh1, r_oh1 = alloc(8)
        oh2, r_oh2 = alloc(8)
        dd, r_dd = alloc(1)
        e2, r_e2 = alloc(1)
        wA, r_wA = alloc(1)
        wB, r_wB = alloc(1)
        A1, r_A1 = alloc(64)
        A2o, r_A2o = alloc(64)
        Aoh, r_Aoh = alloc(64)
        RC, r_RC = alloc(64)
        s1is = [alloc(1, I32) for _ in range(4)]
        s2is = [alloc(1, I32) for _ in range(4)]
        zer, r_zer = alloc(128)
        print("SBUF words used (mixer):", st["off"], "of", AW)

        S.op("act", lambda a: a.activation(out=cst, in_=cvt, func=AF.Silu), r=[r_cvt], w=[r_cs])
        psA, r_psA = psb[7][:, 0:48], ("ps", 7)

        def ada_load(m):
            wv, rw = wa[m % 2]
            S.op("pool", lambda g: g.dma_start(
                out=wv, in_=w_ada[:, m * 1024:(m + 1) * 1024].rearrange("(k p) n -> p k n", p=128)),
                w=[rw], key="k_wa%d" % (m % 2))

        def ada_mm(m):
            wv, rw = wa[m % 2]
            for cc in range(8):
                j = m * 8 + cc
                for k in range(KD):
                    S.op("pe", lambda pe, cc=cc, k=k, j=j: pe.matmul(
                        out=psA[:, j:j + 1], lhsT=wv[:, k, cc * 128:(cc + 1) * 128], rhs=cst[:, k:k + 1],
                        start=(k == 0), stop=(k == KD - 1)), r=[rw, r_cs], w=[r_psA])

        ada_load(0)
        ada_load(1)
        ada_mm(0)
        ada_mm(1)
        S.op("dve", lambda v: v.tensor_tensor(out=sh1, in0=psA[:, 0:8], in1=badT[:, 0:8], op=ALU.add),
             r=[r_psA, r_badT], w=[r_sh1])
        S.op("dve", lambda v: v.scalar_tensor_tensor(out=sc1p, in0=psA[:, 8:16], scalar=1.0, in1=badT[:, 8:16],
                                                     op0=ALU.add, op1=ALU.add), r=[r_psA, r_badT], w=[r_sc1p])

        def make_bcast(dst, rdst, colvec, rcol, dgs):
            for hh in range(2):
                pf, rpf = full()
                for kk in range(4):
                    kc = hh * 4 + kk
                    dg, rdg = dgs[kc % 2]
                    S.op("dve", lambda v, dg=dg, kc=kc: v.tensor_scalar_mul(out=dg, in0=ident, scalar1=colvec[:, kc:kc + 1]),
                         r=[r_ident, rcol], w=[rdg])
                    S.op("pe", lambda pe, dg=dg, kk=kk, pf=pf: pe.matmul(
                        out=pf[:, kk * 128:(kk + 1) * 128], lhsT=ones, rhs=dg, start=True, stop=True),
                        r=[r_ones, rdg], w=[rpf])
                S.op("act", lambda a, hh=hh, pf=pf: a.copy(out=dst[:, hh * 512:(hh + 1) * 512], in_=pf),
                     r=[rpf], w=[rdst])

        def ada_part2():
            for m in range(2, 6):
                ada_mm(m)
                if m + 2 < 6:
                    ada_load(m + 2)
            S.op("dve", lambda v: v.tensor_tensor(out=modT, in0=psA, in1=badT, op=ALU.add),
                 r=[r_psA, r_badT], w=[r_modT])
            S.op("dve", lambda v: v.tensor_scalar_add(out=g1p, in0=modT[:, 16:24], scalar1=1.0), r=[r_modT], w=[r_g1p])
            S.op("dve", lambda v: v.tensor_scalar_add(out=g2p, in0=modT[:, 40:48], scalar1=1.0), r=[r_modT], w=[r_g2p])
            S.op("dve", lambda v: v.scalar_tensor_tensor(out=A2, in0=modT[:, 32:40], scalar=1.0, in1=l1gT,
                                                         op0=ALU.add, op1=ALU.mult), r=[r_modT, r_l1gT], w=[r_A2])
            S.op("dve", lambda v: v.scalar_tensor_tensor(out=B2, in0=modT[:, 32:40], scalar=1.0, in1=l1bT,
                                                         op0=ALU.add, op1=ALU.mult), r=[r_modT, r_l1bT], w=[r_B2])
            S.op("dve", lambda v: v.tensor_tensor(out=B2, in0=B2, in1=modT[:, 24:32], op=ALU.add),
                 r=[r_B2, r_modT], w=[r_B2])
            make_bcast(bc_g1, r_bcg1, g1p, r_g1p, dgs)

        r_win = [("win", q) for q in range(12)]
        win_order = [0, 4, 6, 1, 5, 7, 2, 3, 8, 9, 10, 11]

        def load_win(q):
            S.op("pool", lambda g: g.dma_start(
                out=winv[:, :, q * 512:(q + 1) * 512],
                in_=w_in[:, q * 512:(q + 1) * 512].rearrange("(k p) n -> p k n", p=128)),
                w=[r_win[q]], key="k_win%d" % q)

        for q in win_order[:6]:
            load_win(q)
        ada_load(2)
        ada_load(3)
        S.op("sp", lambda q: q.dma_start(out=wrtv, in_=w_rt.rearrange("(k p) n -> p k n", p=128)),
             w=[r_wrt], key="k_wrt")
        S.op("sp", lambda q: q.dma_start(out=ict, in_=icnt), w=r_ict, key="k_ict")
        S.op("pool", lambda g: g.memset(zer, 1.0e6), w=[r_zer])
        S.op("sp", lambda q: q.dma_start(out=tokbuf.rearrange("(a b) o -> a (b o)", a=128), in_=zer),
             r=[r_zer], w=[("tokbuf",)], key="k_tokz")

        def load_x(i, s):
            buf, rb = xin[s]
            row0 = HL + i * T + s * 128
            S.op("sp", lambda q: q.dma_start(out=buf, in_=xs[row0:row0 + 128, :]), w=[rb], key="k_xin%d" % s)

        def stage_transpose(i):
            for s in range(2):
                buf, rb = xin[s]
                for hh in range(2):
                    pf, rpf = full()
                    for kk in range(4):
                        kc = hh * 4 + kk
                        S.op("pe", lambda pe, kc=kc, kk=kk, pf=pf, buf=buf: pe.transpose(
                            out=pf[:, kk * 128:(kk + 1) * 128], in_=buf[:, kc * 128:(kc + 1) * 128], identity=ident),
                            r=[rb, r_ident], w=[rpf])
                    for kk in range(4):
                        kc = hh * 4 + kk
                        S.op("act", lambda a, kc=kc, kk=kk, pf=pf, s=s: a.activation(
                            out=uTv[:, kc, s * 128:(s + 1) * 128], in_=pf[:, kk * 128:(kk + 1) * 128],
                            func=AF.Identity, bias=sh1[:, kc:kc + 1], scale=sc1p[:, kc:kc + 1]),
                            r=[rpf, r_sh1, r_sc1p], w=[r_uTs[s * 2 + hh]])

        def inproj(cidx, n=T):
            ph, rph = half()
            q = cidx // 4
            for k in range(KD):
                S.op("pe", lambda pe, k=k, ph=ph: pe.matmul(
                    out=ph[:, 0:n], lhsT=winv[:, k, cidx * 128:(cidx + 1) * 128], rhs=uTv[:, k, 0:n],
                    start=(k == 0), stop=(k == KD - 1)), r=[r_win[q]] + r_uTs, w=[rph])
            return ph, rph

        def conv_halo():
            n = HL
            for c in range(KD):
                pv, rpv = inproj(c, n)
                pc, rpc = inproj(16 + c, n)
                av, rav = avalb[c % 2]
                z, rz = zt[c % 2]
                S.op("act", lambda a, av=av, pv=pv: a.copy(out=av[:, 0:n], in_=pv[:, 0:n]), r=[rpv], w=[rav])
                S.op("dve", lambda v, z=z, pc=pc, av=av: v.scalar_tensor_tensor(
                    out=z[:, 0:n], in0=pc[:, 0:n], scalar=hmt[:, 0:1], in1=av[:, 0:n], op0=ALU.mult, op1=ALU.mult),
                    r=[rpc, rav, r_hm], w=[rz])
                S.op("act", lambda a, z=z, c=c: a.copy(out=zcar[:, c * 2:c * 2 + 2], in_=z[:, n - 2:n]),
                     r=[rz], w=[r_zcar])

        def pool_halo():
            for c in range(KD):
                pp, rpp = inproj(24 + c, HL)
                S.op("act", lambda a, pp=pp, c=c: a.activation(
                    out=pcar[:, c * 15:(c + 1) * 15], in_=pp[:, 1:16], func=AF.Identity, scale=hmt[:, 0:1]),
                    r=[rpp, r_hm], w=[r_pcar])

        def inproj_pair(cidx0):
            pp_, rpp_ = pair()
            for ii in range(2):
                cidx = cidx0 + ii
                q = cidx // 4
                for k in range(KD):
                    S.op("pe", lambda pe, k=k, ii=ii, cidx=cidx: pe.matmul(
                        out=pp_[:, ii, :], lhsT=winv[:, k, cidx * 128:(cidx + 1) * 128], rhs=uTv[:, k, :],
                        start=(k == 0), stop=(k == KD - 1)), r=[r_win[q]] + r_uTs, w=[rpp_])
            return pp_, rpp_

        def conv_pair(i, c0):
            cs = (c0, c0 + 1)
            PV, rPV = inproj_pair(c0)
            PC, rPC = inproj_pair(16 + c0)
            PB, rPB = inproj_pair(8 + c0)
            rz = [zt[0][1], zt[1][1]]
            rav = [avalb[0][1], avalb[1][1]]
            rt1 = [ct1[0][1], ct1[1][1]]
            rt2 = [ct2[0][1], ct2[1][1]]
            S.op("act", lambda a: a.copy(out=avp, in_=PV), r=[rPV], w=rav)
            S.op("act", lambda a: a.copy(out=ztp[:, :, 0:2], in_=zcar[:, c0 * 2:(c0 + 2) * 2].rearrange("p (c n) -> p c n", c=2)),
                 r=[r_zcar], w=rz)
            S.op("dve", lambda v: v.tensor_tensor(out=ztp[:, :, 2:T + 2], in0=PC, in1=avp, op=ALU.mult),
                 r=[rPC] + rav, w=rz)
            S.op("act", lambda a: a.copy(out=zcar[:, c0 * 2:(c0 + 2) * 2].rearrange("p (c n) -> p c n", c=2), in_=ztp[:, :, T:T + 2]),
                 r=rz, w=[r_zcar])
            for ii, c in enumerate(cs):
                S.op("act", lambda a, ii=ii, c=c: a.activation(
                    out=t1p[:, ii, :], in_=ztp[:, ii, 2:T + 2], func=AF.Identity, scale=cw[:, c * 3 + 2:c * 3 + 3]),
                    r=[rz[ii], r_cw], w=[rt1[ii]])
            for ii, c in enumerate(cs):
                S.op("dve", lambda v, ii=ii, c=c: v.scalar_tensor_tensor(
                    out=t2p[:, ii, :], in0=ztp[:, ii, 1:T + 1], scalar=cw[:, c * 3 + 1:c * 3 + 2], in1=t1p[:, ii, :],
                    op0=ALU.mult, op1=ALU.add), r=[rz[ii], rt1[ii], r_cw], w=[rt2[ii]])
            for ii, c in enumerate(cs):
                S.op("dve", lambda v, ii=ii, c=c: v.scalar_tensor_tensor(
                    out=t1p[:, ii, :], in0=ztp[:, ii, 0:T], scalar=cw[:, c * 3:c * 3 + 1], in1=t2p[:, ii, :],
                    op0=ALU.mult, op1=ALU.add), r=[rz[ii], rt2[ii], r_cw], w=[rt1[ii]])
            S.op("dve", lambda v: v.tensor_tensor(out=yaTv[:, c0:c0 + 2, :], in0=PB, in1=t1p, op=ALU.mult),
                 r=[rPB] + rt1, w=[r_yaTs[c0 // 2]])

        def pool_pair(i, c0):
            L = T + 15
            w = WINS[c0 // 2]
            gi = c0 // 2
            PP, rPP = inproj_pair(24 + c0)
            rpb = [pbt[0][1], pbt[1][1]]
            S.op("act", lambda a: a.copy(out=pbp[:, :, 0:15], in_=pcar[:, c0 * 15:(c0 + 2) * 15].rearrange("p (c n) -> p c n", c=2)),
                 r=[r_pcar], w=rpb)
            S.op("act", lambda a: a.copy(out=pbp[:, :, 15:L], in_=PP), r=[rPP], w=rpb)
            S.op("act", lambda a: a.copy(out=pcar[:, c0 * 15:(c0 + 2) * 15].rearrange("p (c n) -> p c n", c=2), in_=pbp[:, :, T:L]),
                 r=rpb, w=[r_pcar])
            bufs = [(ptmA, [ptm01[0][1], ptm01[1][1]]), (ptmB, [ptm23[0][1], ptm23[1][1]])]
            cur, rcur = pbp, rpb
            ti = 0
            step, lo = 1, 0
            while step < w:
                nlo = lo + step
                nb, rnb = bufs[ti]
                ti ^= 1
                S.op("dve", lambda v, cur=cur, nb=nb, nlo=nlo, step=step: v.tensor_tensor(
                    out=nb[:, :, nlo:L], in0=cur[:, :, nlo:L], in1=cur[:, :, nlo - step:L - step], op=ALU.add),
                    r=rcur, w=rnb)
                cur, rcur = nb, rnb
                lo, step = nlo, step * 2
            if i == 0:
                nb, rnb = bufs[ti]
                S.op("dve", lambda v, cur=cur, nb=nb: v.tensor_tensor(
                    out=nb[:, :, 15:L], in0=cur[:, :, 15:L],
                    in1=ict[:, gi * T:(gi + 1) * T].unsqueeze(1).to_broadcast([128, 2, T]), op=ALU.mult),
                    r=rcur + r_ict, w=rnb)
                S.op("dve", lambda v, nb=nb: v.tensor_tensor(
                    out=poTv[:, c0:c0 + 2, :], in0=nb[:, :, 15:L], in1=pbp[:, :, 15:L], op=ALU.subtract),
                    r=rnb + rpb, w=[r_poTs[c0 // 2]])
            else:
                S.op("dve", lambda v, cur=cur: v.scalar_tensor_tensor(
                    out=poTv[:, c0:c0 + 2, :], in0=cur[:, :, 15:L], scalar=1.0 / w, in1=pbp[:, :, 15:L],
                    op0=ALU.mult, op1=ALU.subtract), r=rcur + rpb, w=[r_poTs[c0 // 2]])

        def stage_merge(i):
            for e in range(KD):
                pga, rpga = inproj(32 + e)
                pgb, rpgb = inproj(40 + e)
                sa, rsa = sga[e % 2]
                sb, rsb = sgb[e % 2]
                S.op("act", lambda a, sa=sa, pga=pga: a.activation(out=sa, in_=pga, func=AF.Sigmoid), r=[rpga], w=[rsa])
                S.op("act", lambda a, sb=sb, pgb=pgb: a.activation(out=sb, in_=pgb, func=AF.Sigmoid), r=[rpgb], w=[rsb])
                poc, rpoc = half()
                for k in range(KD):
                    S.op("pe", lambda pe, k=k, poc=poc, e=e: pe.matmul(
                        out=poc, lhsT=wocv[:, k, e * 128:(e + 1) * 128], rhs=yaTv[:, k, :],
                        start=(k == 0), stop=(k == KD - 1)), r=[r_woc] + r_yaTs, w=[rpoc])
                ppm, rppm = half()
                g, jj = e // 2, e % 2
                for k in range(2):
                    S.op("pe", lambda pe, k=k, ppm=ppm, g=g, jj=jj: pe.matmul(
                        out=ppm, lhsT=wplv[:, g, k, jj * 128:(jj + 1) * 128], rhs=poTv[:, 2 * g + k, :],
                        start=(k == 0), stop=(k == 1)), r=[r_wpl, r_poTs[g]], w=[rppm])
                m1, rm1 = m1b[e % 2]
                m2, rm2 = m2b[e % 2]
                S.op("dve", lambda v, m1=m1, poc=poc, sa=sa: v.tensor_tensor(out=m1, in0=poc, in1=sa, op=ALU.mult),
                     r=[rpoc, rsa], w=[rm1])
                S.op("dve", lambda v, m2=m2, ppm=ppm, sb=sb, e=e: v.scalar_tensor_tensor(
                    out=m2, in0=ppm, scalar=psc[:, e:e + 1], in1=sb, op0=ALU.mult, op1=ALU.mult),
                    r=[rppm, rsb, r_psc], w=[rm2])
                S.op("dve", lambda g_, m1=m1, m2=m2, e=e: g_.tensor_tensor(out=meTv[:, e, :], in0=m1, in1=m2, op=ALU.add),
                     r=[rm1, rm2], w=[r_meTs[e]])

        def load_xres(i, s):
            buf, rb = xres[s]
            row0 = HL + i * T + s * 128
            S.op("sp", lambda q: q.dma_start(out=buf, in_=xs[row0:row0 + 128, :]), w=[rb], key="k_xres%d" % s)

        def layer_norm_stats(buf, rb, stt, r_stt, mvt, r_mvt, rstd, r_rstd, nb_, r_nb):
            for hh in range(2):
                S.op("dve", lambda v, hh=hh: v.bn_stats(out=stt[:, hh * 6:(hh + 1) * 6], in_=buf[:, hh * 512:(hh + 1) * 512]),
                     r=[rb], w=[r_stt])
            S.op("dve", lambda v: v.bn_aggr(out=mvt, in_=stt), r=[r_stt], w=[r_mvt])
            S.op("act", lambda a: a.activation(out=rstd, in_=mvt[:, 1:2], func=AF.Sqrt, bias=epst[:, 0:1], scale=1.0),
                 r=[r_mvt, r_eps], w=[r_rstd])
            S.op("dve", lambda v: v.reciprocal(out=rstd, in_=rstd), r=[r_rstd], w=[r_rstd])
            S.op("dve", lambda v: v.scalar_tensor_tensor(out=nb_, in0=mvt[:, 0:1], scalar=-1.0, in1=rstd, op0=ALU.mult, op1=ALU.mult),
                 r=[r_mvt, r_rstd], w=[r_nb])

        def stage_wo(i):
            for s in range(2):
                buf, rb = xres[s]
                S.op("act", lambda a, buf=buf: a.mul(out=buf, in_=buf, mul=ALPHA), r=[rb], w=[rb])
                for hh in range(2):
                    pf, rpf = full()
                    for k in range(KD):
                        S.op("pe", lambda pe, k=k, pf=pf, s=s, hh=hh: pe.matmul(
                            out=pf, lhsT=meTv[:, k, s * 128:(s + 1) * 128], rhs=wov[:, k, hh * 512:(hh + 1) * 512],
                            start=(k == 0), stop=(k == KD - 1)), r=[r_wo] + r_meTs, w=[rpf])
                    tv, rtv = tmpv[hh]
                    S.op("dve", lambda v, tv=tv, pf=pf, hh=hh: v.tensor_tensor(
                        out=tv, in0=pf, in1=bc_g1[:, hh * 512:(hh + 1) * 512], op=ALU.mult), r=[rpf, r_bcg1], w=[rtv])
                    S.op("dve", lambda g, tv=tv, buf=buf, hh=hh: g.tensor_tensor(
                        out=buf[:, hh * 512:(hh + 1) * 512], in0=buf[:, hh * 512:(hh + 1) * 512], in1=tv, op=ALU.add),
                        r=[rb, rtv], w=[rb])
                layer_norm_stats(buf, rb, stt, r_stt, mvt, r_mvt, rstd, r_rstd, nbm, r_nbm)
                S.op("act", lambda a, buf=buf: a.activation(out=buf, in_=buf, func=AF.Identity, bias=nbm[:, 0:1], scale=rstd[:, 0:1]),
                     r=[rb, r_nbm, r_rstd], w=[rb])
                j = i * 2 + s
                S.op("sp", lambda q, buf=buf, j=j: q.dma_start(out=n_dram[j * 128:(j + 1) * 128, :], in_=buf),
                     r=[rb], w=[("n_dram", j)], key="k_nst%d" % s)

        def route_A(j, buf, rb):
            for hh in range(2):
                pf, rpf = full()
                for kk in range(4):
                    kc = hh * 4 + kk
                    S.op("pe", lambda pe, kc=kc, kk=kk, pf=pf: pe.transpose(
                        out=pf[:, kk * 128:(kk + 1) * 128], in_=buf[:, kc * 128:(kc + 1) * 128], identity=ident),
                        r=[rb, r_ident], w=[rpf])
                for kk in range(4):
                    kc = hh * 4 + kk
                    S.op("act", lambda a, kc=kc, kk=kk, pf=pf: a.activation(
                        out=hTv[:, kc, :], in_=pf[:, kk * 128:(kk + 1) * 128], func=AF.Identity,
                        bias=B2[:, kc:kc + 1], scale=A2[:, kc:kc + 1]), r=[rpf, r_A2, r_B2], w=[r_hTs[hh]])

        def route_B(j):
            pl, rpl = full()
            for k in range(KD):
                S.op("pe", lambda pe, k=k, pl=pl: pe.matmul(out=pl[:, 0:72], lhsT=hTv[:, k, :], rhs=wrtv[:, k, :],
                                                           start=(k == 0), stop=(k == KD - 1)),
                     r=r_hTs + [r_wrt], w=[rpl])
            v_ = "dve"
            S.op(v_, lambda v: v.tensor_tensor(out=lg, in0=pl[:, 0:72], in1=brt, op=ALU.add), r=[rpl, r_brt], w=[r_lg])
            S.op(v_, lambda v: v.max(out=mx8, in_=lg[:, 0:8]), r=[r_lg], w=[r_mx8])
            S.op(v_, lambda v: v.tensor_scalar(out=gsel, in0=lg[:, 0:8], scalar1=mx8[:, 0:1], scalar2=None, op0=ALU.is_equal),
                 r=[r_lg, r_mx8], w=[r_gsel])
            S.op(v_, lambda v: v.tensor_scalar_mul(out=ngm, in0=mx8[:, 0:1], scalar1=-1.0), r=[r_mx8], w=[r_ngm])
            S.op("act", lambda a: a.activation(out=ex8, in_=lg[:, 0:8], func=AF.Exp, bias=ngm[:, 0:1], scale=1.0),
                 r=[r_lg, r_ngm], w=[r_ex8])
            S.op(v_, lambda v: v.tensor_tensor(
                out=t64.rearrange("p (g e) -> p g e", g=8), in0=lg[:, 8:72].rearrange("p (g e) -> p g e", g=8),
                in1=gsel.unsqueeze(2).to_broadcast([128, 8, 8]), op=ALU.mult), r=[r_lg, r_gsel], w=[r_t64])
            S.op(v_, lambda v: v.tensor_reduce(out=el, in_=t64.rearrange("p (g e) -> p e g", g=8), axis=AX.X, op=ALU.add),
                 r=[r_t64], w=[r_el])
            S.op(v_, lambda v: v.max(out=em8, in_=el), r=[r_el], w=[r_em8])
            S.op(v_, lambda v: v.reduce_sum(out=sme, in_=ex8, axis=AX.X), r=[r_ex8], w=[r_sme])
            S.op(v_, lambda v: v.tensor_scalar(out=oh1, in0=el, scalar1=em8[:, 0:1], scalar2=None, op0=ALU.is_equal),
                 r=[r_el, r_em8], w=[r_oh1])
            S.op(v_, lambda v: v.tensor_scalar(out=oh2, in0=el, scalar1=em8[:, 1:2], scalar2=None, op0=ALU.is_equal),
                 r=[r_el, r_em8], w=[r_oh2])
            S.op(v_, lambda v: v.tensor_tensor(out=dd, in0=em8[:, 1:2], in1=em8[:, 0:1], op=ALU.subtract), r=[r_em8], w=[r_dd])
            S.op("act", lambda a: a.activation(out=e2, in_=dd, func=AF.Exp), r=[r_dd], w=[r_e2])
            S.op(v_, lambda v: v.reciprocal(out=gp, in_=sme), r=[r_sme], w=[r_gp])
            for (Ax, rAx, oh, roh) in ((A1, r_A1, oh1, r_oh1), (A2o, r_A2o, oh2, r_oh2)):
                S.op(v_, lambda v, Ax=Ax, oh=oh: v.tensor_tensor(
                    out=Ax.rearrange("p (g e) -> p g e", g=8), in0=gsel.unsqueeze(2).to_broadcast([128, 8, 8]),
                    in1=oh.unsqueeze(1).to_broadcast([128, 8, 8]), op=ALU.mult), r=[r_gsel, roh], w=[rAx])
            S.op(v_, lambda v: v.tensor_tensor(out=Aoh, in0=A1, in1=A2o, op=ALU.add), r=[r_A1, r_A2o], w=[r_Aoh])
            S.op(v_, lambda v: v.tensor_scalar_add(out=wA, in0=e2, scalar1=1.0), r=[r_e2], w=[r_wA])
            S.op(v_, lambda v: v.reciprocal(out=wA, in_=wA), r=[r_wA], w=[r_wA])
            S.op(v_, lambda v: v.tensor_tensor(out=wB, in0=e2, in1=wA, op=ALU.mult), r=[r_e2, r_wA], w=[r_wB])
            S.op(v_, lambda v: v.tensor_tensor(out=W1[:, j:j + 1], in0=wA, in1=gp, op=ALU.mult), r=[r_wA, r_gp], w=[r_W1])
            S.op(v_, lambda v: v.tensor_tensor(out=W2[:, j:j + 1], in0=wB, in1=gp, op=ALU.mult), r=[r_wB, r_gp], w=[r_W2])

        def route_C(j):
            v_ = "dve"
            pr, rpr = full()
            S.op("pe", lambda pe, pr=pr: pe.matmul(out=pr[:, 0:64], lhsT=ustr, rhs=Aoh, start=True, stop=False),
                 r=[r_ustr, r_Aoh], w=[rpr])
            S.op("pe", lambda pe, pr=pr: pe.matmul(out=pr[:, 0:64], lhsT=ones, rhs=Acc, start=False, stop=True),
                 r=[r_ones, r_Acc], w=[rpr])
            S.op(v_, lambda v, pr=pr: v.scalar_tensor_tensor(out=RC, in0=pr[:, 0:64], scalar=float(CAP - 1), in1=eC,
                                                            op0=ALU.min, op1=ALU.add), r=[rpr, r_eC], w=[r_RC])
            S.op("dve", lambda g: g.tensor_tensor(out=Acc, in0=Acc, in1=Aoh, op=ALU.add), r=[r_Acc, r_Aoh], w=[r_Acc])
            s1i, r_s1i = s1is[j % 4]
            s2i, r_s2i = s2is[j % 4]
            S.op(v_, lambda v: v.tensor_tensor(out=t64, in0=A1, in1=RC, op=ALU.mult), r=[r_A1, r_RC], w=[r_t64])
            S.op(v_, lambda v: v.tensor_tensor(out=t64b, in0=A2o, in1=RC, op=ALU.mult), r=[r_A2o, r_RC], w=[r_t64b])
            S.op(v_, lambda v: v.reduce_sum(out=S1f[:, j:j + 1], in_=t64, axis=AX.X), r=[r_t64], w=[r_S1f])
            S.op(v_, lambda v: v.reduce_sum(out=S2f[:, j:j + 1], in_=t64b, axis=AX.X), r=[r_t64b], w=[r_S2f])
            for (Sf, rSf, si, rsi, kk) in ((S1f, r_S1f, s1i, r_s1i, "k_sc1_%d" % (j % 4)),
                                           (S2f, r_S2f, s2i, r_s2i, "k_sc2_%d" % (j % 4))):
                S.op(v_, lambda v, Sf=Sf, si=si: v.tensor_copy(out=si, in_=Sf[:, j:j + 1]), r=[rSf], w=[rsi])
            for (si, rsi, kk) in ((s1i, r_s1i, "k_sc1_%d" % (j % 4)), (s2i, r_s2i, "k_sc2_%d" % (j % 4))):
                S.op("pool", lambda g, si=si: g.indirect_dma_start(
                    out=tokbuf, out_offset=bass.IndirectOffsetOnAxis(ap=si, axis=0),
                    in_=tokidf[:, j:j + 1], in_offset=None), r=[rsi, r_tokidf, ("tokbuf",)], w=[("tokbuf", j, kk)], key=kk)

        xh, rxh = xin[0]
        S.op("sp", lambda q: q.dma_start(out=xh[0:HL, :], in_=xs[0:HL, :]), w=[rxh], key="k_xin0")
        for hh in range(2):
            pf, rpf = full()
            for kk in range(4):
                kc = hh * 4 + kk
                S.op("pe", lambda pe, kc=kc, kk=kk, pf=pf: pe.transpose(
                    out=pf[:, kk * 128:kk * 128 + HL], in_=xh[0:HL, kc * 128:(kc + 1) * 128], identity=ident[0:HL, 0:HL]),
                    r=[rxh, r_ident], w=[rpf])
            for kk in range(4):
                kc = hh * 4 + kk
                S.op("act", lambda a, kc=kc, kk=kk, pf=pf: a.activation(
                    out=uTv[:, kc, 0:HL], in_=pf[:, kk * 128:kk * 128 + HL], func=AF.Identity,
                    bias=sh1[:, kc:kc + 1], scale=sc1p[:, kc:kc + 1]), r=[rpf, r_sh1, r_sc1p], w=[r_uTs[hh]])
        conv_halo()
        pool_halo()

        for q in win_order[6:]:
            load_win(q)
        S.op("pool", lambda g: g.dma_start(out=wplv, in_=w_pool.rearrange("g (k p) n -> p g k n", p=128)),
             w=[r_wpl], key="k_wpl")

        for s in range(2):
            load_x(0, s)
        stage_transpose(0)
        def precast(e):
            for (dst, src) in ((wgb, w_gate), (wub, w_up), (wdb, w_down)):
                S.op("pool", lambda g, dst=dst, src=src: g.dma_start(
                    out=dst[e].rearrange("(a p) n -> p a n", p=128), in_=src[e].rearrange("(a p) n -> p a n", p=128)),
                    w=[("pre", e, id(dst))], key="k_pre")

        for i in range(NT):
            jp = [(i - 1) * 2, (i - 1) * 2 + 1]
            if i >= 1:
                for e in range((i - 1) * 3, i * 3):
                    precast(e)
                if i in (5, 9, 13):
                    precast(45 + (i - 5) // 4)
            conv_pair(i, 0)
            if i > 0:
                route_A(jp[0], xres[0][0], xres[0][1])
            conv_pair(i, 2)
            if i > 0:
                route_B(jp[0])
            conv_pair(i, 4)
            if i > 0:
                route_A(jp[1], xres[1][0], xres[1][1])
            conv_pair(i, 6)
            if i > 0:
                route_C(jp[0])
            pool_pair(i, 0)
            if i > 0:
                route_B(jp[1])
            pool_pair(i, 2)
            pool_pair(i, 4)
            if i > 0:
                route_C(jp[1])
            pool_pair(i, 6)
            for s in range(2):
                load_xres(i, s)
            if i == 0:
                ada_part2()
                S.op("pool", lambda g: g.dma_start(out=wocv, in_=w_oc.rearrange("(k p) n -> p k n", p=128)),
                     w=[r_woc], key="k_woc")
                S.op("pool", lambda g: g.dma_start(out=wov, in_=w_o.rearrange("(k p) n -> p k n", p=128)),
                     w=[r_wo], key="k_wo")
            stage_merge(i)
            if i + 1 < NT:
                for s in range(2):
                    load_x(i + 1, s)
                stage_transpose(i + 1)
            stage_wo(i)
        for s in range(2):
            j = (NT - 1) * 2 + s
            route_A(j, xres[s][0], xres[s][1])
            route_B(j)
            route_C(j)

        if debug:
            for (src, rs, o0) in ((S1f, r_S1f, 0), (S2f, r_S2f, 1), (W1, r_W1, 2), (W2, r_W2, 3)):
                S.op("sp", lambda q, src=src, o0=o0: q.dma_start(out=dbg[:, o0 * NSUB:(o0 + 1) * NSUB], in_=src),
                     r=[rs], w=[("dbg", o0)], key="k_dbg")

        S.barrier()
        pst["nb"] = 8
        st["off"] = persist_end
        wexp = []
        for sl in range(3):
            a, ra = alloc(KD * DEXP, BF16)
            b, rb_ = alloc(KD * DEXP, BF16)
            c_, rc_ = alloc(4 * D, BF16)
            wexp.append((a.rearrange("p (k n) -> p k n", k=KD), b.rearrange("p (k n) -> p k n", k=KD),
                         c_.rearrange("p (k n) -> p k n", k=4), ra, rb_, rc_))
        xg = [alloc(D), alloc(D), alloc(D), alloc(D)]
        xbT = [alloc(KD * CAP, BF16), alloc(KD * CAP, BF16)]
        sgt = [alloc(CAP), alloc(CAP)]
        actT = [alloc(4 * CAP, BF16), alloc(4 * CAP, BF16)]
        rowsb = [alloc(D), alloc(D), alloc(D)]
        tixf, r_tixf = alloc(128)
        tix, r_tix = alloc(128, I32)
        tkl, r_tkl = alloc(128)
        print("SBUF words used (moe):", st["off"], "of", AW)

        S.op("sp", lambda q: q.dma_start(out=tkl, in_=tokbuf.rearrange("(a b) o -> a (b o)", a=128)),
             r=[("tokbuf",)], w=[r_tkl], key="k_tkl")

        _bregs = {}

        def breg(g, val):
            if val not in _bregs:
                _bregs[val] = g.to_reg(val)
            return _bregs[val]

        def load_expert(e):
            wg_, wu_, wd_, rg, ru, rd = wexp[e % 3]
            kk = "k_wexp%d" % (e % 3)
            if e < NPRE:
                kk = "k_wexph%d" % (e % 3)
                S.op("sp", lambda q: q.dma_start(out=wg_, in_=wgb[e].rearrange("(k p) n -> p k n", p=128)), w=[rg], key=kk)
                S.op("sp", lambda q: q.dma_start(out=wu_, in_=wub[e].rearrange("(k p) n -> p k n", p=128)), w=[ru], key=kk)
                S.op("sp", lambda q: q.dma_start(out=wd_, in_=wdb[e].rearrange("(k p) n -> p k n", p=128)), w=[rd], key=kk)
                return
            S.op("pool", lambda g: g.dma_start(out=wg_, in_=w_gate[e].rearrange("(k p) n -> p k n", p=128)), w=[rg], key=kk)
            S.op("pool", lambda g: g.dma_start(out=wu_, in_=w_up[e].rearrange("(k p) n -> p k n", p=128)), w=[ru], key=kk)
            S.op("pool", lambda g: g.dma_start(out=wd_, in_=w_down[e].rearrange("(k p) n -> p k n", p=128)), w=[rd], key=kk)

        load_expert(0)
        load_expert(1)
        pf, rpf = full()
        S.op("pe", lambda pe, pf=pf: pe.transpose(out=pf[:, 0:128], in_=tkl, identity=ident), r=[r_tkl, r_ident], w=[rpf])
        S.op("dve", lambda v, pf=pf: v.tensor_copy(out=tixf, in_=pf[:, 0:128]), r=[rpf], w=[r_tixf])
        S.op("dve", lambda v: v.tensor_copy(out=tix, in_=tixf), r=[r_tixf], w=[r_tix])
        for (buf_, rb_) in xg:
            S.op("dve", lambda v, buf_=buf_: v.memset(buf_, 0.0), w=[rb_])

        def gather_block(blk):
            buf, rb = xg[blk % 4]
            S.op("pool", lambda g: g.indirect_dma_start(
                out=buf, out_offset=None, in_=n_dram,
                in_offset=bass.IndirectOffsetOnAxis(ap=tix[:, blk:blk + 1], axis=0),
                bounds_check=breg(g, TOK - 1), oob_is_err=False),
                r=[r_tix, ("n_dram",)], w=[rb], key="k_xg%d" % (blk % 4))

        for blk in range(3):
            gather_block(blk)

        def moe_prep(e):
            xb, rxb_ = xbT[e % 2]
            rxbs = [("xb", e % 2, i_) for i_ in range(4)]
            xbv = xb.rearrange("p (k n) -> p k n", k=KD)
            for b in range(2):
                blk = e * 2 + b
                buf, rb = xg[blk % 4]
                for hh in range(2):
                    pf, rpf = full()
                    for kk in range(4):
                        kc = hh * 4 + kk
                        S.op("pe", lambda pe, kc=kc, kk=kk, pf=pf, buf=buf: pe.transpose(
                            out=pf[:, kk * 128:(kk + 1) * 128], in_=buf[:, kc * 128:(kc + 1) * 128], identity=ident),
                            r=[rb, r_ident], w=[rpf])
                    for kk in range(4):
                        kc = hh * 4 + kk
                        if hh == 0:
                            S.op("act", lambda a, kc=kc, kk=kk, pf=pf, b=b: a.activation(
                                out=xbv[:, kc, b * 128:(b + 1) * 128], in_=pf[:, kk * 128:(kk + 1) * 128], func=AF.Identity,
                                bias=B2[:, kc:kc + 1], scale=A2[:, kc:kc + 1]), r=[rpf, r_A2, r_B2], w=[rxbs[b * 2 + hh]])
                        else:
                            S.op("dve", lambda v, kc=kc, kk=kk, pf=pf, b=b: v.tensor_scalar(
                                out=xbv[:, kc, b * 128:(b + 1) * 128], in0=pf[:, kk * 128:(kk + 1) * 128],
                                scalar1=A2[:, kc:kc + 1], scalar2=B2[:, kc:kc + 1], op0=ALU.mult, op1=ALU.add),
                                r=[rpf, r_A2, r_B2], w=[rxbs[b * 2 + hh]])
                if blk + 3 < NBLK:
                    gather_block(blk + 3)

        def moe_ffn(e):
            wg_, wu_, wd_, rg, ru, rd = wexp[e % 3]
            xb, rxb_ = xbT[e % 2]
            rxbs = [("xb", e % 2, i_) for i_ in range(4)]
            xbv = xb.rearrange("p (k n) -> p k n", k=KD)
            at, rat_ = actT[e % 2]
            rats = [("actT", e % 2, i_) for i_ in range(4)]
            atv = at.rearrange("p (k n) -> p k n", k=4)
            for f in range(4):
                pg, rpg = half()
                for k in range(KD):
                    S.op("pe", lambda pe, k=k, pg=pg, f=f: pe.matmul(
                        out=pg, lhsT=wg_[:, k, f * 128:(f + 1) * 128], rhs=xbv[:, k, :],
                        start=(k == 0), stop=(k == KD - 1)), r=[rg] + rxbs, w=[rpg])
                pu, rpu = half()
                for k in range(KD):
                    S.op("pe", lambda pe, k=k, pu=pu, f=f: pe.matmul(
                        out=pu, lhsT=wu_[:, k, f * 128:(f + 1) * 128], rhs=xbv[:, k, :],
                        start=(k == 0), stop=(k == KD - 1)), r=[ru] + rxbs, w=[rpu])
                sg_, rsg = sgt[f % 2]
                S.op("act", lambda a, sg_=sg_, pg=pg: a.activation(out=sg_, in_=pg, func=AF.Silu), r=[rpg], w=[rsg])
                S.op("dve", lambda v, sg_=sg_, pu=pu, f=f: v.tensor_tensor(out=atv[:, f, :], in0=pu, in1=sg_, op=ALU.mult),
                     r=[rpu, rsg], w=[rats[f]])
            for b in range(2):
                blk = e * 2 + b
                rsb_, rrs_ = rowsb[blk % 3]
                rrsh = [("rowsb", blk % 3, 0), ("rowsb", blk % 3, 1)]
                for hh in range(2):
                    pf, rpf = full()
                    for f in range(4):
                        S.op("pe", lambda pe, f=f, pf=pf, b=b, hh=hh: pe.matmul(
                            out=pf, lhsT=atv[:, f, b * 128:(b + 1) * 128], rhs=wd_[:, f, hh * 512:(hh + 1) * 512],
                            start=(f == 0), stop=(f == 3)), r=[rd] + rats, w=[rpf])
                    if hh == 0:
                        S.op("act", lambda a, pf=pf, rsb_=rsb_: a.copy(out=rsb_[:, 0:512], in_=pf), r=[rpf], w=[rrsh[0]])
                    else:
                        S.op("dve", lambda v, pf=pf, rsb_=rsb_: v.tensor_copy(out=rsb_[:, 512:1024], in_=pf), r=[rpf], w=[rrsh[1]])
                S.op("sp", lambda q, rsb_=rsb_, blk=blk: q.dma_start(out=rows_dram[blk * 128:(blk + 1) * 128, :], in_=rsb_),
                     r=rrsh, w=[("rows", blk)], key="k_rows%d" % (blk % 3))

        moe_prep(0)
        for e in range(NEXP):
            if e + 2 < NEXP:
                load_expert(e + 2)
            if e + 1 < NEXP:
                moe_prep(e + 1)
            moe_ffn(e)

        S.barrier()
        st["off"] = persist_end
        bc_g2, r_bcg2 = alloc(D)
        bl1g, r_bl1g = alloc(D)
        bl1b, r_bl1b = alloc(D)
        bl2g, r_bl2g = alloc(D)
        bl2b, r_bl2b = alloc(D)
        dgs2 = [alloc(128), alloc(128)]
        S1i, r_S1i = alloc(NSUB, I32)
        S2i, r_S2i = alloc(NSUB, I32)
        stt2, r_stt2 = alloc(12)
        mvt2, r_mvt2 = alloc(2)
        rstd2, r_rstd2 = alloc(1)
        nb2, r_nb2 = alloc(1)
        NBUF = 6
        r1b = [alloc(D) for _ in range(NBUF)]
        r2b = [alloc(D) for _ in range(NBUF)]
        nrb = [alloc(D) for _ in range(NBUF)]
        for (dst, rd, src, kk) in ((bl1g, r_bl1g, ln1g, "k_b1"), (bl1b, r_bl1b, ln1b, "k_b2"),
                                   (bl2g, r_bl2g, ln2g, "k_b3"), (bl2b, r_bl2b, ln2b, "k_b4")):
            S.op("sp", lambda q, dst=dst, src=src: q.dma_start(out=dst, in_=src.partition_broadcast(128)), w=[rd], key=kk)
        S.op("dve", lambda v: v.tensor_copy(out=S1i, in_=S1f), r=[r_S1f], w=[r_S1i])
        S.op("dve", lambda v: v.tensor_copy(out=S2i, in_=S2f), r=[r_S2f], w=[r_S2i])
        S.op("act", lambda a: a.mul(out=bl1g, in_=bl1g, mul=ALPHA), r=[r_bl1g], w=[r_bl1g])
        S.op("act", lambda a: a.mul(out=bl1b, in_=bl1b, mul=ALPHA), r=[r_bl1b], w=[r_bl1b])

        def issue_loads(j):
            r1, rr1 = r1b[j % NBUF]
            r2, rr2 = r2b[j % NBUF]
            nr, rnr = nrb[j % NBUF]
            S.op("sp", lambda q: q.dma_start(out=nr, in_=n_dram[j * 128:(j + 1) * 128, :]), r=[("n_dram",)], w=[rnr],
                 key="k_nr%d" % (j % NBUF))
            S.op("pool", lambda g: g.indirect_dma_start(
                out=r1, out_offset=None, in_=rows_dram, in_offset=bass.IndirectOffsetOnAxis(ap=S1i[:, j:j + 1], axis=0)),
                r=[r_S1i, ("rows",)], w=[rr1], key="k_r1%d" % (j % NBUF))
            S.op("pool", lambda g: g.indirect_dma_start(
                out=r2, out_offset=None, in_=rows_dram, in_offset=bass.IndirectOffsetOnAxis(ap=S2i[:, j:j + 1], axis=0)),
                r=[r_S2i, ("rows",)], w=[rr2], key="k_r2%d" % (j % NBUF))

        issue_loads(0)
        issue_loads(1)
        issue_loads(2)
        make_bcast(bc_g2, r_bcg2, g2p, r_g2p, dgs2)

        def stage1(j):
            r1, rr1 = r1b[j % NBUF]
            r2, rr2 = r2b[j % NBUF]
            nr, rnr = nrb[j % NBUF]
            S.op("pool", lambda g: g.tensor_tensor(out=nr, in0=nr, in1=bl1g, op=ALU.mult), r=[rnr, r_bl1g], w=[rnr])
            S.op("pool", lambda g: g.tensor_tensor(out=nr, in0=nr, in1=bl1b, op=ALU.add), r=[rnr, r_bl1b], w=[rnr])
            S.op("act", lambda a: a.activation(out=r1, in_=r1, func=AF.Identity, scale=W1[:, j:j + 1]),
                 r=[rr1, r_W1], w=[rr1])
            S.op("dve", lambda v: v.scalar_tensor_tensor(
                out=r2, in0=r2, scalar=W2[:, j:j + 1], in1=r1, op0=ALU.mult, op1=ALU.add), r=[rr2, rr1, r_W2], w=[rr2])
            S.op("dve", lambda v: v.tensor_tensor(out=r2, in0=r2, in1=bc_g2, op=ALU.mult), r=[rr2, r_bcg2], w=[rr2])
            S.op("pool", lambda g: g.tensor_tensor(out=r2, in0=r2, in1=nr, op=ALU.add), r=[rnr, rr2], w=[rr2])

        def stage2(j):
            r1, rr1 = r1b[j % NBUF]
            r2, rr2 = r2b[j % NBUF]
            layer_norm_stats(r2, rr2, stt2, r_stt2, mvt2, r_mvt2, rstd2, r_rstd2, nb2, r_nb2)
            S.op("act", lambda a: a.activation(out=r1, in_=r2, func=AF.Identity, bias=nb2[:, 0:1], scale=rstd2[:, 0:1]),
                 r=[rr2, r_nb2, r_rstd2], w=[rr1])
            S.op("dve", lambda v: v.tensor_tensor(out=r1, in0=r1, in1=bl2g, op=ALU.mult), r=[rr1, r_bl2g], w=[rr1])
            S.op("dve", lambda v: v.tensor_tensor(out=r1, in0=r1, in1=bl2b, op=ALU.add), r=[rr1, r_bl2b], w=[rr1])
            S.op("sp", lambda q: q.dma_start(out=out[j * 128:(j + 1) * 128, :], in_=r1), r=[rr1], w=[("out", j)],
                 key="k_out%d" % (j % NBUF))

        stage1(0)
        for j in range(NSUB):
            if j + 3 < NSUB:
                issue_loads(j + 3)
            if j + 1 < NSUB:
                stage1(j + 1)
            stage2(j)

        keys = sorted(S.dma_cnt.keys())
        S.number()
        with contextlib.ExitStack() as es:
            engsem = {e: es.enter_context(nc.semaphore("s_" + e)) for e in ("pe", "act", "dve", "pool")}
            dmasem = {k: es.enter_context(nc.semaphore(k)) for k in keys}
            block = es.enter_context(nc.Block())

            @block.tensor
            def _(pe):
                S.emit_engine("pe", pe, engsem, dmasem)

            @block.scalar
            def _(a):
                S.emit_engine("act", a, engsem, dmasem)

            @block.vector
            def _(v):
                S.emit_engine("dve", v, engsem, dmasem)

            @block.gpsimd
            def _(g):
                S.emit_engine("pool", g, engsem, dmasem)

            @block.sync
            def _(q):
                S.emit_engine("sp", q, engsem, dmasem)
    print("ops:", S.n, {e: len(S.ops[e]) for e in ENGS}, "dma keys:", len(keys))
    return nc


_CACHE = {}


def _prep_inputs(inputs):
    f = lambda a: np.ascontiguousarray(np.asarray(a, dtype=np.float32))
    x = f(inputs["x"])
    c = f(inputs["c"])
    col = lambda v: np.ascontiguousarray(f(v).reshape(-1, 128).T)
    shared = {
        "w_ada": f(inputs["w_ada"][0]),
        "b_adaT": col(inputs["b_ada"][0]),
        "w_in": f(inputs["w_in"][0]),
        "cwT": np.ascontiguousarray(f(inputs["conv_w"][0]).reshape(3, KD, 128).transpose(2, 1, 0).reshape(128, KD * 3)),
        "w_oc": f(inputs["w_out_conv"][0]),
        "w_pool": f(inputs["w_pool"][0]),
        "pscT": col(inputs["pool_scale"][0]),
        "w_o": f(inputs["w_o"][0]),
        "ln1gT": col(inputs["ln1_g"][0]),
        "ln1bT": col(inputs["ln1_b"][0]),
        "ln1g": f(inputs["ln1_g"][0]),
        "ln1b": f(inputs["ln1_b"][0]),
        "ln2g": f(inputs["ln2_g"][0]),
        "ln2b": f(inputs["ln2_b"][0]),
        "w_rt": np.ascontiguousarray(np.concatenate([f(inputs["w_group"][0]), f(inputs["w_router"][0])], axis=1)),
        "b_rt": np.ascontiguousarray(np.concatenate([f(inputs["b_group"][0]).reshape(-1), f(inputs["b_router"][0]).reshape(-1)])),
        "w_gate": f(inputs["w_gate"][0]),
        "w_up": f(inputs["w_up"][0]),
        "w_down": f(inputs["w_down"][0]),
    }
    in_maps = []
    for r in range(NCORES):
        b, hf = r // 2, r % 2
        t0 = hf * TOK
        halo = x[b, t0 - HL:t0] if hf else np.zeros((HL, D), np.float32)
        tt = np.arange(T) + t0 + 1
        ic = np.concatenate([1.0 / np.minimum(tt, w) for w in WINS]).astype(np.float32)
        m = dict(shared)
        m["xs"] = np.ascontiguousarray(np.concatenate([halo, x[b, t0:t0 + TOK]], axis=0))
        m["cvec"] = col(c[b])
        m["hm"] = np.full((128, 1), float(hf), np.float32)
        m["icnt"] = np.ascontiguousarray(np.broadcast_to(ic[None, :], (128, 4 * T)))
        in_maps.append(m)
    return in_maps


def kernel(**inputs):
    if "nc" not in _CACHE:
        _CACHE["nc"] = build_program()
    nc = _CACHE["nc"]
    in_maps = _prep_inputs(inputs)
    res = run_bass_kernel_spmd(nc, in_maps, core_ids=list(range(NCORES)))
    outs = [np.asarray(res.results[r]["out"], dtype=np.float32) for r in range(NCORES)]
    return np.concatenate(outs, axis=0).reshape(4, SEQ, D)
```

```python
import contextlib
import numpy as np
import concourse.bass as bass
import concourse.mybir as mybir
from concourse.bass_utils import run_bass_kernel_spmd

F32 = mybir.dt.float32
BF16 = mybir.dt.bfloat16
I32 = mybir.dt.int32
AF = mybir.ActivationFunctionType
ALU = mybir.AluOpType
AX = mybir.AxisListType

NCORES = 8
D = 1024
KD = 8
SEQ = 8192
TOK = 4096
HL = 16
T = 256
NT = TOK // T
NSUB = TOK // 128
NEXP = 64
DEXP = 512
CAP = 256
NBLK = NEXP * CAP // 128
WINS = (2, 4, 8, 16)
ALPHA = 2.0 ** 0.25
EPS = 1e-5
NCOLS = 6144
NPRE = 52

ENGS = ("pe", "act", "dve", "pool", "sp")


class _Op:
    __slots__ = ("eng", "fn", "deps", "key", "signal", "idx", "dmaval")

    def __init__(self, eng, fn, key):
        self.eng = eng
        self.fn = fn
        self.key = key
        self.deps = []
        self.signal = False
        self.idx = 0
        self.dmaval = 0


class Sched:
    def __init__(self):
        self.ops = {e: [] for e in ENGS}
        self.last_w = {}
        self.readers = {}
        self.dma_cnt = {}
        self.bar = {}
        self.n = 0

    def op(self, eng, fn, r=(), w=(), key=None):
        o = _Op(eng, fn, key)
        deps = {}
        if eng in self.bar:
            for d in self.bar.pop(eng):
                deps[id(d[1]) if d[0] == "op" else d[1]] = d
        for res in r:
            lw = self.last_w.get(res)
            if lw is not None:
                deps[id(lw)] = ("op", lw)
            if res[0] == "ps":
                for rd in self.readers.get(res, ()):
                    if rd.eng != eng:
                        deps[id(rd)] = ("op", rd)
        for res in w:
            lw = self.last_w.get(res)
            if lw is not None:
                deps[id(lw)] = ("op", lw)
            for rd in self.readers.get(res, ()):
                deps[id(rd)] = ("op", rd)
        has_reader_dep = any(d[0] == "op" and d[1].key is None for d in deps.values())
        for d in deps.values():
            if d[0] == "dma":
                o.deps.append(d)
                continue
            p = d[1]
            if p is o:
                continue
            if p.key is not None:
                if key == p.key and has_reader_dep:
                    continue
                o.deps.append(("dma", p.key, self.dma_cnt[p.key] * 16))
            else:
                if eng == "pe" and p.eng == "pe":
                    continue
                p.signal = True
                o.deps.append(("op", p))
        if key is not None:
            self.dma_cnt[key] = self.dma_cnt.get(key, 0) + 1
            o.dmaval = self.dma_cnt[key] * 16
        for res in w:
            self.last_w[res] = o
            self.readers[res] = []
        for res in r:
            self.readers.setdefault(res, []).append(o)
        self.ops[eng].append(o)
        self.n += 1
        return o

    def barrier(self):
        deps = []
        for e in ("pe", "act", "dve", "pool"):
            if self.ops[e]:
                for o in reversed(self.ops[e]):
                    if o.key is None:
                        o.signal = True
                        deps.append(("op", o))
                        break
        for k, c in self.dma_cnt.items():
            deps.append(("dma", k, c * 16))
        for e in ENGS:
            self.bar[e] = list(deps)
        self.last_w = {}
        self.readers = {}

    def number(self):
        for e in ENGS:
            i = 0
            for o in self.ops[e]:
                if o.key is None and o.signal:
                    i += 1
                    o.idx = i

    def emit_engine(self, e, eng, engsem, dmasem):
        waited = {}
        for o in self.ops[e]:
            need = {}
            for d in o.deps:
                if d[0] == "dma":
                    sem, val = dmasem[d[1]], d[2]
                else:
                    sem, val = engsem[d[1].eng], d[1].idx
                if need.get(id(sem), (None, 0))[1] < val:
                    need[id(sem)] = (sem, val)
            for sem, val in need.values():
                if waited.get(id(sem), 0) < val:
                    eng.wait_ge(sem, val)
                    waited[id(sem)] = val
            ins = o.fn(eng)
            if o.key is not None:
                ins.then_inc(dmasem[o.key], 16)
            elif o.signal:
                ins.then_inc(engsem[e], 1)
        if e == "sp":
            for k, c in self.dma_cnt.items():
                eng.wait_ge(dmasem[k], c * 16)


def build_program(debug=False):
    nc = bass.Bass("TRN2", target_bir_lowering=False)

    def din(name, shape, dt=F32):
        return nc.dram_tensor(name, list(shape), dt, kind="ExternalInput").ap()

    xs = din("xs", [HL + TOK, D])
    cvec = din("cvec", [128, KD])
    hm = din("hm", [128, 1])
    icnt = din("icnt", [128, 4 * T])
    w_ada = din("w_ada", [D, NCOLS])
    b_adaT = din("b_adaT", [128, 48])
    w_in = din("w_in", [D, NCOLS])
    cwT = din("cwT", [128, KD * 3])
    w_oc = din("w_oc", [D, D])
    w_pool = din("w_pool", [4, 256, 256])
    pscT = din("pscT", [128, KD])
    w_o = din("w_o", [D, D])
    ln1gT = din("ln1gT", [128, KD])
    ln1bT = din("ln1bT", [128, KD])
    ln1g = din("ln1g", [D])
    ln1b = din("ln1b", [D])
    ln2g = din("ln2g", [D])
    ln2b = din("ln2b", [D])
    w_rt = din("w_rt", [D, 72])
    b_rt = din("b_rt", [72])
    w_gate = din("w_gate", [NEXP, D, DEXP])
    w_up = din("w_up", [NEXP, D, DEXP])
    w_down = din("w_down", [NEXP, DEXP, D])
    out = nc.dram_tensor("out", [TOK, D], F32, kind="ExternalOutput").ap()
    skind = "ExternalOutput" if debug else "Internal"
    n_dram = nc.dram_tensor("n_dram", [TOK, D], F32, kind=skind).ap()
    rows_dram = nc.dram_tensor("rows_dram", [NEXP * CAP, D], F32, kind="Internal").ap()
    tokbuf = nc.dram_tensor("tokbuf", [NEXP * CAP, 1], F32, kind=skind).ap()
    wgb = nc.dram_tensor("wgb", [NPRE, D, DEXP], BF16, kind="Internal").ap()
    wub = nc.dram_tensor("wub", [NPRE, D, DEXP], BF16, kind="Internal").ap()
    wdb = nc.dram_tensor("wdb", [NPRE, DEXP, D], BF16, kind="Internal").ap()
    if debug:
        dbg = nc.dram_tensor("dbg", [128, 4 * NSUB], F32, kind="ExternalOutput").ap()

    S = Sched()
    AW = 53150
    with (
        nc.sbuf_tensor("arena", [128, AW], F32) as arena,
        nc.psum_tensor("ps0", [128, 512], F32) as ps0,
        nc.psum_tensor("ps1", [128, 512], F32) as ps1,
        nc.psum_tensor("ps2", [128, 512], F32) as ps2,
        nc.psum_tensor("ps3", [128, 512], F32) as ps3,
        nc.psum_tensor("ps4", [128, 512], F32) as ps4,
        nc.psum_tensor("ps5", [128, 512], F32) as ps5,
        nc.psum_tensor("ps6", [128, 512], F32) as ps6,
        nc.psum_tensor("ps7", [128, 512], F32) as ps7,
    ):
        psb = [ps0, ps1, ps2, ps3, ps4, ps5, ps6, ps7]
        st = {"off": 0, "uid": 0}

        def alloc(n, dt=F32):
            words = n if dt in (F32, I32) else (n + 1) // 2
            words = (words + 1) // 2 * 2
            a = arena[:, st["off"]:st["off"] + words]
            st["off"] += words
            assert st["off"] <= AW, st["off"]
            st["uid"] += 1
            if dt != F32:
                a = a.bitcast(dt)
            a = a[:, 0:n]
            return a, ("sb", st["uid"])

        def alloc_pair(n):
            n2 = (n + 1) // 2 * 2
            off = st["off"]
            a, ra = alloc(n)
            b, rb = alloc(n)
            pv = arena[:, off:off + 2 * n2].rearrange("p (c n) -> p c n", c=2)[:, :, 0:n]
            return pv, [(a, ra), (b, rb)]

        pst = {"i": 0, "nb": 7}

        def half():
            n2 = pst["nb"] * 2
            i = pst["i"] % n2
            pst["i"] += 1
            b, hh = i // 2, i % 2
            return psb[b][:, hh * 256:(hh + 1) * 256], ("ps", b)

        def pair():
            p, r = full()
            return p.rearrange("p (c n) -> p c n", c=2), r

        def full():
            if pst["i"] % 2:
                pst["i"] += 1
            n2 = pst["nb"] * 2
            b = (pst["i"] % n2) // 2
            pst["i"] += 2
            return psb[b][:, :], ("ps", b)

        ident, r_ident = alloc(128)
        ustr, r_ustr = alloc(128)
        ones, r_ones = alloc(128)
        eC, r_eC = alloc(64)
        eCi, r_eCi = alloc(64, I32)
        tokidi, r_tokidi = alloc(NSUB, I32)
        tokidf, r_tokidf = alloc(NSUB)
        epst, r_eps = alloc(1)
        cvt, r_cvt = alloc(KD)
        cst, r_cs = alloc(KD, BF16)
        modT, r_modT = alloc(48)
        badT, r_badT = alloc(48)
        sh1, r_sh1 = alloc(KD)
        sc1p, r_sc1p = alloc(KD)
        A2, r_A2 = alloc(KD)
        B2, r_B2 = alloc(KD)
        g1p, r_g1p = alloc(KD)
        g2p, r_g2p = alloc(KD)
        l1gT, r_l1gT = alloc(KD)
        l1bT, r_l1bT = alloc(KD)
        cw, r_cw = alloc(KD * 3)
        psc, r_psc = alloc(KD)
        hmt, r_hm = alloc(1)
        brt, r_brt = alloc(72)
        S1f, r_S1f = alloc(NSUB)
        S2f, r_S2f = alloc(NSUB)
        W1, r_W1 = alloc(NSUB)
        W2, r_W2 = alloc(NSUB)
        Acc, r_Acc = alloc(64)
        persist_end = st["off"]

        S.op("pool", lambda g: g.memset(ident, 1.0), w=[r_ident])
        S.op("pool", lambda g: g.affine_select(out=ident, in_=ident, pattern=[[-1, 128]], compare_op=ALU.is_equal,
                                               fill=0.0, base=0, channel_multiplier=1), r=[r_ident], w=[r_ident])
        S.op("pool", lambda g: g.memset(ustr, 1.0), w=[r_ustr])
        S.op("pool", lambda g: g.affine_select(out=ustr, in_=ustr, pattern=[[1, 128]], compare_op=ALU.is_gt,
                                               fill=0.0, base=0, channel_multiplier=-1), r=[r_ustr], w=[r_ustr])
        S.op("pool", lambda g: g.memset(ones, 1.0), w=[r_ones])
        S.op("pool", lambda g: g.memset(epst, EPS), w=[r_eps])
        S.op("pool", lambda g: g.memset(Acc, 0.0), w=[r_Acc])
        S.op("pool", lambda g: g.iota(eCi, pattern=[[CAP, 64]], base=0, channel_multiplier=0), w=[r_eCi])
        S.op("pool", lambda g: g.tensor_copy(out=eC, in_=eCi), r=[r_eCi], w=[r_eC])
        S.op("pool", lambda g: g.iota(tokidi, pattern=[[128, NSUB]], base=0, channel_multiplier=1), w=[r_tokidi])
        S.op("pool", lambda g: g.tensor_copy(out=tokidf, in_=tokidi), r=[r_tokidi], w=[r_tokidf])

        def ld(dst, rdst, src, key):
            S.op("sp", lambda q: q.dma_start(out=dst, in_=src), w=[rdst], key=key)

        ld(cvt, r_cvt, cvec, "k_cvt")
        ld(badT, r_badT, b_adaT, "k_bad")
        ld(l1gT, r_l1gT, ln1gT, "k_l1g")
        ld(l1bT, r_l1bT, ln1bT, "k_l1b")
        ld(cw, r_cw, cwT, "k_cw")
        ld(psc, r_psc, pscT, "k_psc")
        ld(hmt, r_hm, hm, "k_hm")
        ld(brt, r_brt, b_rt.partition_broadcast(128), "k_brt")

        winb, _ = alloc(KD * NCOLS, BF16)
        winv = winb.rearrange("p (k n) -> p k n", k=KD)
        wocb, r_woc = alloc(KD * D, BF16)
        wocv = wocb.rearrange("p (k n) -> p k n", k=KD)
        wob, r_wo = alloc(KD * D, BF16)
        wov = wob.rearrange("p (k n) -> p k n", k=KD)
        wa = [(wocv, r_woc), (wov, r_wo)]
        wplb, r_wpl = alloc(4 * 2 * 256, BF16)
        wplv = wplb.rearrange("p (g k n) -> p g k n", g=4, k=2)
        wrt, r_wrt = alloc(KD * 72)
        wrtv = wrt.rearrange("p (k n) -> p k n", k=KD)
        bc_g1, r_bcg1 = alloc(D)
        xin = [alloc(D), alloc(D)]
        xres = [alloc(D), alloc(D)]
        uT, _r = alloc(KD * T, BF16)
        r_uTs = [("uT", i_) for i_ in range(4)]
        uTv = uT.rearrange("p (k n) -> p k n", k=KD)
        ztp, zt = alloc_pair(T + 2)
        zcar, r_zcar = alloc(KD * 2)
        avp, avalb = alloc_pair(T)
        t1p, ct1 = alloc_pair(T)
        t2p, ct2 = alloc_pair(T)
        ct = ct1 + ct2
        pbp, pbt = alloc_pair(T + 15)
        pcar, r_pcar = alloc(KD * 15)
        ptmA, ptm01 = alloc_pair(T + 15)
        ptmB, ptm23 = alloc_pair(T + 15)
        ptm = ptm01 + ptm23
        stt, r_stt = alloc(12)
        mvt, r_mvt = alloc(2)
        rstd, r_rstd = alloc(1)
        nbm, r_nbm = alloc(1)
        tmpv = [alloc(512), alloc(512)]
        ict = arena[:, st["off"] - 1024:st["off"]]
        r_ict = [tmpv[0][1], tmpv[1][1]]
        dgs = [(ct1[0][0][:, 0:128], ct1[0][1]), (ct1[1][0][:, 0:128], ct1[1][1])]
        yaT, _r = alloc(KD * T, BF16)
        r_yaTs = [("yaT", i_) for i_ in range(4)]
        yaTv = yaT.rearrange("p (k n) -> p k n", k=KD)
        poT, _r = alloc(KD * T, BF16)
        r_poTs = [("poT", i_) for i_ in range(4)]
        poTv = poT.rearrange("p (k n) -> p k n", k=KD)
        meT, _r = alloc(KD * T, BF16)
        r_meTs = [("meT", i_) for i_ in range(8)]
        meTv = meT.rearrange("p (k n) -> p k n", k=KD)
        sga = [alloc(T), alloc(T)]
        sgb = [alloc(T), alloc(T)]
        m1b = [alloc(T), alloc(T)]
        m2b = [alloc(T)] * 2
        hT, r_hT = alloc(KD * 128)
        r_hTs = [("hT", 0), ("hT", 1)]
        hTv = hT.rearrange("p (k n) -> p k n", k=KD)
        lg, r_lg = alloc(72)
        mx8, r_mx8 = alloc(8)
        gsel, r_gsel = alloc(8)
        ngm, r_ngm = alloc(1)
        ex8, r_ex8 = alloc(8)
        sme, r_sme = alloc(1)
        gp, r_gp = alloc(1)
        t64, r_t64 = alloc(64)
        t64b, r_t64b = alloc(64)
        el, r_el = alloc(8)
        em8, r_em8 = alloc(8)
        oh1, r_oh1 = alloc(8)
        oh2, r_oh2 = alloc(8)
        dd, r_dd = alloc(1)
        e2, r_e2 = alloc(1)
        wA, r_wA = alloc(1)
        wB, r_wB = alloc(1)
        A1, r_A1 = alloc(64)
        A2o, r_A2o = alloc(64)
        Aoh, r_Aoh = alloc(64)
        RC, r_RC = alloc(64)
        s1is = [alloc(1, I32) for _ in range(4)]
        s2is = [alloc(1, I32) for _ in range(4)]
        zer, r_zer = alloc(128)
        print("SBUF words used (mixer):", st["off"], "of", AW)

        S.op("act", lambda a: a.activation(out=cst, in_=cvt, func=AF.Silu), r=[r_cvt], w=[r_cs])
        psA, r_psA = psb[7][:, 0:48], ("ps", 7)

        def ada_load(m):
            wv, rw = wa[m % 2]
            S.op("pool", lambda g: g.dma_start(
                out=wv, in_=w_ada[:, m * 1024:(m + 1) * 1024].rearrange("(k p) n -> p k n", p=128)),
                w=[rw], key="k_wa%d" % (m % 2))

        def ada_mm(m):
            wv, rw = wa[m % 2]
            for cc in range(8):
                j = m * 8 + cc
                for k in range(KD):
                    S.op("pe", lambda pe, cc=cc, k=k, j=j: pe.matmul(
                        out=psA[:, j:j + 1], lhsT=wv[:, k, cc * 128:(cc + 1) * 128], rhs=cst[:, k:k + 1],
                        start=(k == 0), stop=(k == KD - 1)), r=[rw, r_cs], w=[r_psA])

        ada_load(0)
        ada_load(1)
        ada_mm(0)
        ada_mm(1)
        S.op("dve", lambda v: v.tensor_tensor(out=sh1, in0=psA[:, 0:8], in1=badT[:, 0:8], op=ALU.add),
             r=[r_psA, r_badT], w=[r_sh1])
        S.op("dve", lambda v: v.scalar_tensor_tensor(out=sc1p, in0=psA[:, 8:16], scalar=1.0, in1=badT[:, 8:16],
                                                     op0=ALU.add, op1=ALU.add), r=[r_psA, r_badT], w=[r_sc1p])

        def make_bcast(dst, rdst, colvec, rcol, dgs):
            for hh in range(2):
                pf, rpf = full()
                for kk in range(4):
                    kc = hh * 4 + kk
                    dg, rdg = dgs[kc % 2]
                    S.op("dve", lambda v, dg=dg, kc=kc: v.tensor_scalar_mul(out=dg, in0=ident, scalar1=colvec[:, kc:kc + 1]),
                         r=[r_ident, rcol], w=[rdg])
                    S.op("pe", lambda pe, dg=dg, kk=kk, pf=pf: pe.matmul(
                        out=pf[:, kk * 128:(kk + 1) * 128], lhsT=ones, rhs=dg, start=True, stop=True),
                        r=[r_ones, rdg], w=[rpf])
                S.op("act", lambda a, hh=hh, pf=pf: a.copy(out=dst[:, hh * 512:(hh + 1) * 512], in_=pf),
                     r=[rpf], w=[rdst])

        def ada_part2():
            for m in range(2, 6):
                ada_mm(m)
                if m + 2 < 6:
                    ada_load(m + 2)
            S.op("dve", lambda v: v.tensor_tensor(out=modT, in0=psA, in1=badT, op=ALU.add),
                 r=[r_psA, r_badT], w=[r_modT])
            S.op("dve", lambda v: v.tensor_scalar_add(out=g1p, in0=modT[:, 16:24], scalar1=1.0), r=[r_modT], w=[r_g1p])
            S.op("dve", lambda v: v.tensor_scalar_add(out=g2p, in0=modT[:, 40:48], scalar1=1.0), r=[r_modT], w=[r_g2p])
            S.op("dve", lambda v: v.scalar_tensor_tensor(out=A2, in0=modT[:, 32:40], scalar=1.0, in1=l1gT,
                                                         op0=ALU.add, op1=ALU.mult), r=[r_modT, r_l1gT], w=[r_A2])
            S.op("dve", lambda v: v.scalar_tensor_tensor(out=B2, in0=modT[:, 32:40], scalar=1.0, in1=l1bT,
                                                         op0=ALU.add, op1=ALU.mult), r=[r_modT, r_l1bT], w=[r_B2])
            S.op("dve", lambda v: v.tensor_tensor(out=B2, in0=B2, in1=modT[:, 24:32], op=ALU.add),
                 r=[r_B2, r_modT], w=[r_B2])
            make_bcast(bc_g1, r_bcg1, g1p, r_g1p, dgs)

        r_win = [("win", q) for q in range(12)]
        win_order = [0, 4, 6, 1, 5, 7, 2, 3, 8, 9, 10, 11]

        def load_win(q):
            S.op("pool", lambda g: g.dma_start(
                out=winv[:, :, q * 512:(q + 1) * 512],
                in_=w_in[:, q * 512:(q + 1) * 512].rearrange("(k p) n -> p k n", p=128)),
                w=[r_win[q]], key="k_win%d" % q)

        for q in win_order[:6]:
            load_win(q)
        ada_load(2)
        ada_load(3)
        S.op("sp", lambda q: q.dma_start(out=wrtv, in_=w_rt.rearrange("(k p) n -> p k n", p=128)),
             w=[r_wrt], key="k_wrt")
        S.op("sp", lambda q: q.dma_start(out=ict, in_=icnt), w=r_ict, key="k_ict")
        S.op("pool", lambda g: g.memset(zer, 1.0e6), w=[r_zer])
        S.op("sp", lambda q: q.dma_start(out=tokbuf.rearrange("(a b) o -> a (b o)", a=128), in_=zer),
             r=[r_zer], w=[("tokbuf",)], key="k_tokz")

        def load_x(i, s):
            buf, rb = xin[s]
            row0 = HL + i * T + s * 128
            S.op("sp", lambda q: q.dma_start(out=buf, in_=xs[row0:row0 + 128, :]), w=[rb], key="k_xin%d" % s)

        def stage_transpose(i):
            for s in range(2):
                buf, rb = xin[s]
                for hh in range(2):
                    pf, rpf = full()
                    for kk in range(4):
                        kc = hh * 4 + kk
                        S.op("pe", lambda pe, kc=kc, kk=kk, pf=pf, buf=buf: pe.transpose(
                            out=pf[:, kk * 128:(kk + 1) * 128], in_=buf[:, kc * 128:(kc + 1) * 128], identity=ident),
                            r=[rb, r_ident], w=[rpf])
                    for kk in range(4):
                        kc = hh * 4 + kk
                        S.op("act", lambda a, kc=kc, kk=kk, pf=pf, s=s: a.activation(
                            out=uTv[:, kc, s * 128:(s + 1) * 128], in_=pf[:, kk * 128:(kk + 1) * 128],
                            func=AF.Identity, bias=sh1[:, kc:kc + 1], scale=sc1p[:, kc:kc + 1]),
                            r=[rpf, r_sh1, r_sc1p], w=[r_uTs[s * 2 + hh]])

        def inproj(cidx, n=T):
            ph, rph = half()
            q = cidx // 4
            for k in range(KD):
                S.op("pe", lambda pe, k=k, ph=ph: pe.matmul(
                    out=ph[:, 0:n], lhsT=winv[:, k, cidx * 128:(cidx + 1) * 128], rhs=uTv[:, k, 0:n],
                    start=(k == 0), stop=(k == KD - 1)), r=[r_win[q]] + r_uTs, w=[rph])
            return ph, rph

        def conv_halo():
            n = HL
            for c in range(KD):
                pv, rpv = inproj(c, n)
                pc, rpc = inproj(16 + c, n)
                av, rav = avalb[c % 2]
                z, rz = zt[c % 2]
                S.op("act", lambda a, av=av, pv=pv: a.copy(out=av[:, 0:n], in_=pv[:, 0:n]), r=[rpv], w=[rav])
                S.op("dve", lambda v, z=z, pc=pc, av=av: v.scalar_tensor_tensor(
                    out=z[:, 0:n], in0=pc[:, 0:n], scalar=hmt[:, 0:1], in1=av[:, 0:n], op0=ALU.mult, op1=ALU.mult),
                    r=[rpc, rav, r_hm], w=[rz])
                S.op("act", lambda a, z=z, c=c: a.copy(out=zcar[:, c * 2:c * 2 + 2], in_=z[:, n - 2:n]),
                     r=[rz], w=[r_zcar])

        def pool_halo():
            for c in range(KD):
                pp, rpp = inproj(24 + c, HL)
                S.op("act", lambda a, pp=pp, c=c: a.activation(
                    out=pcar[:, c * 15:(c + 1) * 15], in_=pp[:, 1:16], func=AF.Identity, scale=hmt[:, 0:1]),
                    r=[rpp, r_hm], w=[r_pcar])

        def inproj_pair(cidx0):
            pp_, rpp_ = pair()
            for ii in range(2):
                cidx = cidx0 + ii
                q = cidx // 4
                for k in range(KD):
                    S.op("pe", lambda pe, k=k, ii=ii, cidx=cidx: pe.matmul(
                        out=pp_[:, ii, :], lhsT=winv[:, k, cidx * 128:(cidx + 1) * 128], rhs=uTv[:, k, :],
                        start=(k == 0), stop=(k == KD - 1)), r=[r_win[q]] + r_uTs, w=[rpp_])
            return pp_, rpp_

        def conv_pair(i, c0):
            cs = (c0, c0 + 1)
            PV, rPV = inproj_pair(c0)
            PC, rPC = inproj_pair(16 + c0)
            PB, rPB = inproj_pair(8 + c0)
            rz = [zt[0][1], zt[1][1]]
            rav = [avalb[0][1], avalb[1][1]]
            rt1 = [ct1[0][1], ct1[1][1]]
            rt2 = [ct2[0][1], ct2[1][1]]
            S.op("act", lambda a: a.copy(out=avp, in_=PV), r=[rPV], w=rav)
            S.op("act", lambda a: a.copy(out=ztp[:, :, 0:2], in_=zcar[:, c0 * 2:(c0 + 2) * 2].rearrange("p (c n) -> p c n", c=2)),
                 r=[r_zcar], w=rz)
            S.op("dve", lambda v: v.tensor_tensor(out=ztp[:, :, 2:T + 2], in0=PC, in1=avp, op=ALU.mult),
                 r=[rPC] + rav, w=rz)
            S.op("act", lambda a: a.copy(out=zcar[:, c0 * 2:(c0 + 2) * 2].rearrange("p (c n) -> p c n", c=2), in_=ztp[:, :, T:T + 2]),
                 r=rz, w=[r_zcar])
            for ii, c in enumerate(cs):
                S.op("act", lambda a, ii=ii, c=c: a.activation(
                    out=t1p[:, ii, :], in_=ztp[:, ii, 2:T + 2], func=AF.Identity, scale=cw[:, c * 3 + 2:c * 3 + 3]),
                    r=[rz[ii], r_cw], w=[rt1[ii]])
            for ii, c in enumerate(cs):
                S.op("dve", lambda v, ii=ii, c=c: v.scalar_tensor_tensor(
                    out=t2p[:, ii, :], in0=ztp[:, ii, 1:T + 1], scalar=cw[:, c * 3 + 1:c * 3 + 2], in1=t1p[:, ii, :],
                    op0=ALU.mult, op1=ALU.add), r=[rz[ii], rt1[ii], r_cw], w=[rt2[ii]])
            for ii, c in enumerate(cs):
                S.op("dve", lambda v, ii=ii, c=c: v.scalar_tensor_tensor(
                    out=t1p[:, ii, :], in0=ztp[:, ii, 0:T], scalar=cw[:, c * 3:c * 3 + 1], in1=t2p[:, ii, :],
                    op0=ALU.mult, op1=ALU.add), r=[rz[ii], rt2[ii], r_cw], w=[rt1[ii]])
            S.op("dve", lambda v: v.tensor_tensor(out=yaTv[:, c0:c0 + 2, :], in0=PB, in1=t1p, op=ALU.mult),
                 r=[rPB] + rt1, w=[r_yaTs[c0 // 2]])

        def pool_pair(i, c0):
            L = T + 15
            w = WINS[c0 // 2]
            gi = c0 // 2
            PP, rPP = inproj_pair(24 + c0)
            rpb = [pbt[0][1], pbt[1][1]]
            S.op("act", lambda a: a.copy(out=pbp[:, :, 0:15], in_=pcar[:, c0 * 15:(c0 + 2) * 15].rearrange("p (c n) -> p c n", c=2)),
                 r=[r_pcar], w=rpb)
            S.op("act", lambda a: a.copy(out=pbp[:, :, 15:L], in_=PP), r=[rPP], w=rpb)
            S.op("act", lambda a: a.copy(out=pcar[:, c0 * 15:(c0 + 2) * 15].rearrange("p (c n) -> p c n", c=2), in_=pbp[:, :, T:L]),
                 r=rpb, w=[r_pcar])
            bufs = [(ptmA, [ptm01[0][1], ptm01[1][1]]), (ptmB, [ptm23[0][1], ptm23[1][1]])]
            cur, rcur = pbp, rpb
            ti = 0
            step, lo = 1, 0
            while step < w:
                nlo = lo + step
                nb, rnb = bufs[ti]
                ti ^= 1
                S.op("dve", lambda v, cur=cur, nb=nb, nlo=nlo, step=step: v.tensor_tensor(
                    out=nb[:, :, nlo:L], in0=cur[:, :, nlo:L], in1=cur[:, :, nlo - step:L - step], op=ALU.add),
                    r=rcur, w=rnb)
                cur, rcur = nb, rnb
                lo, step = nlo, step * 2
            if i == 0:
                nb, rnb = bufs[ti]
                S.op("dve", lambda v, cur=cur, nb=nb: v.tensor_tensor(
                    out=nb[:, :, 15:L], in0=cur[:, :, 15:L],
                    in1=ict[:, gi * T:(gi + 1) * T].unsqueeze(1).to_broadcast([128, 2, T]), op=ALU.mult),
                    r=rcur + r_ict, w=rnb)
                S.op("dve", lambda v, nb=nb: v.tensor_tensor(
                    out=poTv[:, c0:c0 + 2, :], in0=nb[:, :, 15:L], in1=pbp[:, :, 15:L], op=ALU.subtract),
                    r=rnb + rpb, w=[r_poTs[c0 // 2]])
            else:
                S.op("dve", lambda v, cur=cur: v.scalar_tensor_tensor(
                    out=poTv[:, c0:c0 + 2, :], in0=cur[:, :, 15:L], scalar=1.0 / w, in1=pbp[:, :, 15:L],
                    op0=ALU.mult, op1=ALU.subtract), r=rcur + rpb, w=[r_poTs[c0 // 2]])

        def stage_merge(i):
            for e in range(KD):
                pga, rpga = inproj(32 + e)
                pgb, rpgb = inproj(40 + e)
                sa, rsa = sga[e % 2]
                sb, rsb = sgb[e % 2]
                S.op("act", lambda a, sa=sa, pga=pga: a.activation(out=sa, in_=pga, func=AF.Sigmoid), r=[rpga], w=[rsa])
                S.op("act", lambda a, sb=sb, pgb=pgb: a.activation(out=sb, in_=pgb, func=AF.Sigmoid), r=[rpgb], w=[rsb])
                poc, rpoc = half()
                for k in range(KD):
                    S.op("pe", lambda pe, k=k, poc=poc, e=e: pe.matmul(
                        out=poc, lhsT=wocv[:, k, e * 128:(e + 1) * 128], rhs=yaTv[:, k, :],
                        start=(k == 0), stop=(k == KD - 1)), r=[r_woc] + r_yaTs, w=[rpoc])
                ppm, rppm = half()
                g, jj = e // 2, e % 2
                for k in range(2):
                    S.op("pe", lambda pe, k=k, ppm=ppm, g=g, jj=jj: pe.matmul(
                        out=ppm, lhsT=wplv[:, g, k, jj * 128:(jj + 1) * 128], rhs=poTv[:, 2 * g + k, :],
                        start=(k == 0), stop=(k == 1)), r=[r_wpl, r_poTs[g]], w=[rppm])
                m1, rm1 = m1b[e % 2]
                m2, rm2 = m2b[e % 2]
                S.op("dve", lambda v, m1=m1, poc=poc, sa=sa: v.tensor_tensor(out=m1, in0=poc, in1=sa, op=ALU.mult),
                     r=[rpoc, rsa], w=[rm1])
                S.op("dve", lambda v, m2=m2, ppm=ppm, sb=sb, e=e: v.scalar_tensor_tensor(
                    out=m2, in0=ppm, scalar=psc[:, e:e + 1], in1=sb, op0=ALU.mult, op1=ALU.mult),
                    r=[rppm, rsb, r_psc], w=[rm2])
                S.op("dve", lambda g_, m1=m1, m2=m2, e=e: g_.tensor_tensor(out=meTv[:, e, :], in0=m1, in1=m2, op=ALU.add),
                     r=[rm1, rm2], w=[r_meTs[e]])

        def load_xres(i, s):
            buf, rb = xres[s]
            row0 = HL + i * T + s * 128
            S.op("sp", lambda q: q.dma_start(out=buf, in_=xs[row0:row0 + 128, :]), w=[rb], key="k_xres%d" % s)

        def layer_norm_stats(buf, rb, stt, r_stt, mvt, r_mvt, rstd, r_rstd, nb_, r_nb):
            for hh in range(2):
                S.op("dve", lambda v, hh=hh: v.bn_stats(out=stt[:, hh * 6:(hh + 1) * 6], in_=buf[:, hh * 512:(hh + 1) * 512]),
                     r=[rb], w=[r_stt])
            S.op("dve", lambda v: v.bn_aggr(out=mvt, in_=stt), r=[r_stt], w=[r_mvt])
            S.op("act", lambda a: a.activation(out=rstd, in_=mvt[:, 1:2], func=AF.Sqrt, bias=epst[:, 0:1], scale=1.0),
                 r=[r_mvt, r_eps], w=[r_rstd])
            S.op("dve", lambda v: v.reciprocal(out=rstd, in_=rstd), r=[r_rstd], w=[r_rstd])
            S.op("dve", lambda v: v.scalar_tensor_tensor(out=nb_, in0=mvt[:, 0:1], scalar=-1.0, in1=rstd, op0=ALU.mult, op1=ALU.mult),
                 r=[r_mvt, r_rstd], w=[r_nb])

        def stage_wo(i):
            for s in range(2):
                buf, rb = xres[s]
                S.op("act", lambda a, buf=buf: a.mul(out=buf, in_=buf, mul=ALPHA), r=[rb], w=[rb])
                for hh in range(2):
                    pf, rpf = full()
                    for k in range(KD):
                        S.op("pe", lambda pe, k=k, pf=pf, s=s, hh=hh: pe.matmul(
                            out=pf, lhsT=meTv[:, k, s * 128:(s + 1) * 128], rhs=wov[:, k, hh * 512:(hh + 1) * 512],
                            start=(k == 0), stop=(k == KD - 1)), r=[r_wo] + r_meTs, w=[rpf])
                    tv, rtv = tmpv[hh]
                    S.op("dve", lambda v, tv=tv, pf=pf, hh=hh: v.tensor_tensor(
                        out=tv, in0=pf, in1=bc_g1[:, hh * 512:(hh + 1) * 512], op=ALU.mult), r=[rpf, r_bcg1], w=[rtv])
                    S.op("dve", lambda g, tv=tv, buf=buf, hh=hh: g.tensor_tensor(
                        out=buf[:, hh * 512:(hh + 1) * 512], in0=buf[:, hh * 512:(hh + 1) * 512], in1=tv, op=ALU.add),
                        r=[rb, rtv], w=[rb])
                layer_norm_stats(buf, rb, stt, r_stt, mvt, r_mvt, rstd, r_rstd, nbm, r_nbm)
                S.op("act", lambda a, buf=buf: a.activation(out=buf, in_=buf, func=AF.Identity, bias=nbm[:, 0:1], scale=rstd[:, 0:1]),
                     r=[rb, r_nbm, r_rstd], w=[rb])
                j = i * 2 + s
                S.op("sp", lambda q, buf=buf, j=j: q.dma_start(out=n_dram[j * 128:(j + 1) * 128, :], in_=buf),
                     r=[rb], w=[("n_dram", j)], key="k_nst%d" % s)

        def route_A(j, buf, rb):
            for hh in range(2):
                pf, rpf = full()
                for kk in range(4):
                    kc = hh * 4 + kk
                    S.op("pe", lambda pe, kc=kc, kk=kk, pf=pf: pe.transpose(
                        out=pf[:, kk * 128:(kk + 1) * 128], in_=buf[:, kc * 128:(kc + 1) * 128], identity=ident),
                        r=[rb, r_ident], w=[rpf])
                for kk in range(4):
                    kc = hh * 4 + kk
                    S.op("act", lambda a, kc=kc, kk=kk, pf=pf: a.activation(
                        out=hTv[:, kc, :], in_=pf[:, kk * 128:(kk + 1) * 128], func=AF.Identity,
                        bias=B2[:, kc:kc + 1], scale=A2[:, kc:kc + 1]), r=[rpf, r_A2, r_B2], w=[r_hTs[hh]])

        def route_B(j):
            pl, rpl = full()
            for k in range(KD):
                S.op("pe", lambda pe, k=k, pl=pl: pe.matmul(out=pl[:, 0:72], lhsT=hTv[:, k, :], rhs=wrtv[:, k, :],
                                                           start=(k == 0), stop=(k == KD - 1)),
                     r=r_hTs + [r_wrt], w=[rpl])
            v_ = "dve"
            S.op(v_, lambda v: v.tensor_tensor(out=lg, in0=pl[:, 0:72], in1=brt, op=ALU.add), r=[rpl, r_brt], w=[r_lg])
            S.op(v_, lambda v: v.max(out=mx8, in_=lg[:, 0:8]), r=[r_lg], w=[r_mx8])
            S.op(v_, lambda v: v.tensor_scalar(out=gsel, in0=lg[:, 0:8], scalar1=mx8[:, 0:1], scalar2=None, op0=ALU.is_equal),
                 r=[r_lg, r_mx8], w=[r_gsel])
            S.op(v_, lambda v: v.tensor_scalar_mul(out=ngm, in0=mx8[:, 0:1], scalar1=-1.0), r=[r_mx8], w=[r_ngm])
            S.op("act", lambda a: a.activation(out=ex8, in_=lg[:, 0:8], func=AF.Exp, bias=ngm[:, 0:1], scale=1.0),
                 r=[r_lg, r_ngm], w=[r_ex8])
            S.op(v_, lambda v: v.tensor_tensor(
                out=t64.rearrange("p (g e) -> p g e", g=8), in0=lg[:, 8:72].rearrange("p (g e) -> p g e", g=8),
                in1=gsel.unsqueeze(2).to_broadcast([128, 8, 8]), op=ALU.mult), r=[r_lg, r_gsel], w=[r_t64])
            S.op(v_, lambda v: v.tensor_reduce(out=el, in_=t64.rearrange("p (g e) -> p e g", g=8), axis=AX.X, op=ALU.add),
                 r=[r_t64], w=[r_el])
            S.op(v_, lambda v: v.max(out=em8, in_=el), r=[r_el], w=[r_em8])
            S.op(v_, lambda v: v.reduce_sum(out=sme, in_=ex8, axis=AX.X), r=[r_ex8], w=[r_sme])
            S.op(v_, lambda v: v.tensor_scalar(out=oh1, in0=el, scalar1=em8[:, 0:1], scalar2=None, op0=ALU.is_equal),
                 r=[r_el, r_em8], w=[r_oh1])
            S.op(v_, lambda v: v.tensor_scalar(out=oh2, in0=el, scalar1=em8[:, 1:2], scalar2=None, op0=ALU.is_equal),
                 r=[r_el, r_em8], w=[r_oh2])
            S.op(v_, lambda v: v.tensor_tensor(out=dd, in0=em8[:, 1:2], in1=em8[:, 0:1], op=ALU.subtract), r=[r_em8], w=[r_dd])
            S.op("act", lambda a: a.activation(out=e2, in_=dd, func=AF.Exp), r=[r_dd], w=[r_e2])
            S.op(v_, lambda v: v.reciprocal(out=gp, in_=sme), r=[r_sme], w=[r_gp])
            for (Ax, rAx, oh, roh) in ((A1, r_A1, oh1, r_oh1), (A2o, r_A2o, oh2, r_oh2)):
                S.op(v_, lambda v, Ax=Ax, oh=oh: v.tensor_tensor(
                    out=Ax.rearrange("p (g e) -> p g e", g=8), in0=gsel.unsqueeze(2).to_broadcast([128, 8, 8]),
                    in1=oh.unsqueeze(1).to_broadcast([128, 8, 8]), op=ALU.mult), r=[r_gsel, roh], w=[rAx])
            S.op(v_, lambda v: v.tensor_tensor(out=Aoh, in0=A1, in1=A2o, op=ALU.add), r=[r_A1, r_A2o], w=[r_Aoh])
            S.op(v_, lambda v: v.tensor_scalar_add(out=wA, in0=e2, scalar1=1.0), r=[r_e2], w=[r_wA])
            S.op(v_, lambda v: v.reciprocal(out=wA, in_=wA), r=[r_wA], w=[r_wA])
            S.op(v_, lambda v: v.tensor_tensor(out=wB, in0=e2, in1=wA, op=ALU.mult), r=[r_e2, r_wA], w=[r_wB])
            S.op(v_, lambda v: v.tensor_tensor(out=W1[:, j:j + 1], in0=wA, in1=gp, op=ALU.mult), r=[r_wA, r_gp], w=[r_W1])
            S.op(v_, lambda v: v.tensor_tensor(out=W2[:, j:j + 1], in0=wB, in1=gp, op=ALU.mult), r=[r_wB, r_gp], w=[r_W2])

        def route_C(j):
            v_ = "dve"
            pr, rpr = full()
            S.op("pe", lambda pe, pr=pr: pe.matmul(out=pr[:, 0:64], lhsT=ustr, rhs=Aoh, start=True, stop=False),
                 r=[r_ustr, r_Aoh], w=[rpr])
            S.op("pe", lambda pe, pr=pr: pe.matmul(out=pr[:, 0:64], lhsT=ones, rhs=Acc, start=False, stop=True),
                 r=[r_ones, r_Acc], w=[rpr])
            S.op(v_, lambda v, pr=pr: v.scalar_tensor_tensor(out=RC, in0=pr[:, 0:64], scalar=float(CAP - 1), in1=eC,
                                                            op0=ALU.min, op1=ALU.add), r=[rpr, r_eC], w=[r_RC])
            S.op("dve", lambda g: g.tensor_tensor(out=Acc, in0=Acc, in1=Aoh, op=ALU.add), r=[r_Acc, r_Aoh], w=[r_Acc])
            s1i, r_s1i = s1is[j % 4]
            s2i, r_s2i = s2is[j % 4]
            S.op(v_, lambda v: v.tensor_tensor(out=t64, in0=A1, in1=RC, op=ALU.mult), r=[r_A1, r_RC], w=[r_t64])
            S.op(v_, lambda v: v.tensor_tensor(out=t64b, in0=A2o, in1=RC, op=ALU.mult), r=[r_A2o, r_RC], w=[r_t64b])
            S.op(v_, lambda v: v.reduce_sum(out=S1f[:, j:j + 1], in_=t64, axis=AX.X), r=[r_t64], w=[r_S1f])
            S.op(v_, lambda v: v.reduce_sum(out=S2f[:, j:j + 1], in_=t64b, axis=AX.X), r=[r_t64b], w=[r_S2f])
            for (Sf, rSf, si, rsi, kk) in ((S1f, r_S1f, s1i, r_s1i, "k_sc1_%d" % (j % 4)),
                                           (S2f, r_S2f, s2i, r_s2i, "k_sc2_%d" % (j % 4))):
                S.op(v_, lambda v, Sf=Sf, si=si: v.tensor_copy(out=si, in_=Sf[:, j:j + 1]), r=[rSf], w=[rsi])
            for (si, rsi, kk) in ((s1i, r_s1i, "k_sc1_%d" % (j % 4)), (s2i, r_s2i, "k_sc2_%d" % (j % 4))):
                S.op("pool", lambda g, si=si: g.indirect_dma_start(
                    out=tokbuf, out_offset=bass.IndirectOffsetOnAxis(ap=si, axis=0),
                    in_=tokidf[:, j:j + 1], in_offset=None), r=[rsi, r_tokidf, ("tokbuf",)], w=[("tokbuf", j, kk)], key=kk)

        xh, rxh = xin[0]
        S.op("sp", lambda q: q.dma_start(out=xh[0:HL, :], in_=xs[0:HL, :]), w=[rxh], key="k_xin0")
        for hh in range(2):
            pf, rpf = full()
            for kk in range(4):
                kc = hh * 4 + kk
                S.op("pe", lambda pe, kc=kc, kk=kk, pf=pf: pe.transpose(
                    out=pf[:, kk * 128:kk * 128 + HL], in_=xh[0:HL, kc * 128:(kc + 1) * 128], identity=ident[0:HL, 0:HL]),
                    r=[rxh, r_ident], w=[rpf])
            for kk in range(4):
                kc = hh * 4 + kk
                S.op("act", lambda a, kc=kc, kk=kk, pf=pf: a.activation(
                    out=uTv[:, kc, 0:HL], in_=pf[:, kk * 128:kk * 128 + HL], func=AF.Identity,
                    bias=sh1[:, kc:kc + 1], scale=sc1p[:, kc:kc + 1]), r=[rpf, r_sh1, r_sc1p], w=[r_uTs[hh]])
        conv_halo()
        pool_halo()

        for q in win_order[6:]:
            load_win(q)
        S.op("pool", lambda g: g.dma_start(out=wplv, in_=w_pool.rearrange("g (k p) n -> p g k n", p=128)),
             w=[r_wpl], key="k_wpl")

        for s in range(2):
            load_x(0, s)
        stage_transpose(0)
        def precast(e):
            for (dst, src) in ((wgb, w_gate), (wub, w_up), (wdb, w_down)):
                S.op("pool", lambda g, dst=dst, src=src: g.dma_start(
                    out=dst[e].rearrange("(a p) n -> p a n", p=128), in_=src[e].rearrange("(a p) n -> p a n", p=128)),
                    w=[("pre", e, id(dst))], key="k_pre")

        for i in range(NT):
            jp = [(i - 1) * 2, (i - 1) * 2 + 1]
            if i >= 1:
                for e in range((i - 1) * 3, i * 3):
                    precast(e)
                if i % 2 == 1 and i >= 3:
                    precast(45 + (i - 3) // 2)
            conv_pair(i, 0)
            if i > 0:
                route_A(jp[0], xres[0][0], xres[0][1])
            conv_pair(i, 2)
            if i > 0:
                route_B(jp[0])
            conv_pair(i, 4)
            if i > 0:
                route_A(jp[1], xres[1][0], xres[1][1])
            conv_pair(i, 6)
            if i > 0:
                route_C(jp[0])
            pool_pair(i, 0)
            if i > 0:
                route_B(jp[1])
            pool_pair(i, 2)
            pool_pair(i, 4)
            if i > 0:
                route_C(jp[1])
            pool_pair(i, 6)
            for s in range(2):
                load_xres(i, s)
            if i == 0:
                ada_part2()
                S.op("pool", lambda g: g.dma_start(out=wocv, in_=w_oc.rearrange("(k p) n -> p k n", p=128)),
                     w=[r_woc], key="k_woc")
                S.op("pool", lambda g: g.dma_start(out=wov, in_=w_o.rearrange("(k p) n -> p k n", p=128)),
                     w=[r_wo], key="k_wo")
            stage_merge(i)
            if i + 1 < NT:
                for s in range(2):
                    load_x(i + 1, s)
                stage_transpose(i + 1)
            stage_wo(i)
        for s in range(2):
            j = (NT - 1) * 2 + s
            route_A(j, xres[s][0], xres[s][1])
            route_B(j)
            route_C(j)

        if debug:
            for (src, rs, o0) in ((S1f, r_S1f, 0), (S2f, r_S2f, 1), (W1, r_W1, 2), (W2, r_W2, 3)):
                S.op("sp", lambda q, src=src, o0=o0: q.dma_start(out=dbg[:, o0 * NSUB:(o0 + 1) * NSUB], in_=src),
                     r=[rs], w=[("dbg", o0)], key="k_dbg")

        S.barrier()
        pst["nb"] = 8
        st["off"] = persist_end
        wexp = []
        for sl in range(3):
            a, ra = alloc(KD * DEXP, BF16)
            b, rb_ = alloc(KD * DEXP, BF16)
            c_, rc_ = alloc(4 * D, BF16)
            wexp.append((a.rearrange("p (k n) -> p k n", k=KD), b.rearrange("p (k n) -> p k n", k=KD),
                         c_.rearrange("p (k n) -> p k n", k=4), ra, rb_, rc_))
        xg = [alloc(D), alloc(D), alloc(D), alloc(D)]
        xbT = [alloc(KD * CAP, BF16), alloc(KD * CAP, BF16)]
        sgt = [alloc(CAP), alloc(CAP)]
        actT = [alloc(4 * CAP, BF16), alloc(4 * CAP, BF16)]
        rowsb = [alloc(D), alloc(D), alloc(D)]
        tixf, r_tixf = alloc(128)
        tix, r_tix = alloc(128, I32)
        tkl, r_tkl = alloc(128)
        print("SBUF words used (moe):", st["off"], "of", AW)

        S.op("sp", lambda q: q.dma_start(out=tkl, in_=tokbuf.rearrange("(a b) o -> a (b o)", a=128)),
             r=[("tokbuf",)], w=[r_tkl], key="k_tkl")

        _bregs = {}

        def breg(g, val):
            if val not in _bregs:
                _bregs[val] = g.to_reg(val)
            return _bregs[val]

        def load_expert(e):
            wg_, wu_, wd_, rg, ru, rd = wexp[e % 3]
            kk = "k_wexp%d" % (e % 3)
            if e < NPRE:
                kk = "k_wexph%d" % (e % 3)
                S.op("sp", lambda q: q.dma_start(out=wg_, in_=wgb[e].rearrange("(k p) n -> p k n", p=128)), w=[rg], key=kk)
                S.op("sp", lambda q: q.dma_start(out=wu_, in_=wub[e].rearrange("(k p) n -> p k n", p=128)), w=[ru], key=kk)
                S.op("sp", lambda q: q.dma_start(out=wd_, in_=wdb[e].rearrange("(k p) n -> p k n", p=128)), w=[rd], key=kk)
                return
            S.op("pool", lambda g: g.dma_start(out=wg_, in_=w_gate[e].rearrange("(k p) n -> p k n", p=128)), w=[rg], key=kk)
            S.op("pool", lambda g: g.dma_start(out=wu_, in_=w_up[e].rearrange("(k p) n -> p k n", p=128)), w=[ru], key=kk)
            S.op("pool", lambda g: g.dma_start(out=wd_, in_=w_down[e].rearrange("(k p) n -> p k n", p=128)), w=[rd], key=kk)

        load_expert(0)
        load_expert(1)
        pf, rpf = full()
        S.op("pe", lambda pe, pf=pf: pe.transpose(out=pf[:, 0:128], in_=tkl, identity=ident), r=[r_tkl, r_ident], w=[rpf])
        S.op("dve", lambda v, pf=pf: v.tensor_copy(out=tixf, in_=pf[:, 0:128]), r=[rpf], w=[r_tixf])
        S.op("dve", lambda v: v.tensor_copy(out=tix, in_=tixf), r=[r_tixf], w=[r_tix])
        for (buf_, rb_) in xg:
            S.op("dve", lambda v, buf_=buf_: v.memset(buf_, 0.0), w=[rb_])

        def gather_block(blk):
            buf, rb = xg[blk % 4]
            S.op("pool", lambda g: g.indirect_dma_start(
                out=buf, out_offset=None, in_=n_dram,
                in_offset=bass.IndirectOffsetOnAxis(ap=tix[:, blk:blk + 1], axis=0),
                bounds_check=breg(g, TOK - 1), oob_is_err=False),
                r=[r_tix, ("n_dram",)], w=[rb], key="k_xg%d" % (blk % 4))

        for blk in range(3):
            gather_block(blk)

        def moe_prep(e):
            xb, rxb_ = xbT[e % 2]
            rxbs = [("xb", e % 2, i_) for i_ in range(4)]
            xbv = xb.rearrange("p (k n) -> p k n", k=KD)
            for b in range(2):
                blk = e * 2 + b
                buf, rb = xg[blk % 4]
                for hh in range(2):
                    pf, rpf = full()
                    for kk in range(4):
                        kc = hh * 4 + kk
                        S.op("pe", lambda pe, kc=kc, kk=kk, pf=pf, buf=buf: pe.transpose(
                            out=pf[:, kk * 128:(kk + 1) * 128], in_=buf[:, kc * 128:(kc + 1) * 128], identity=ident),
                            r=[rb, r_ident], w=[rpf])
                    for kk in range(4):
                        kc = hh * 4 + kk
                        if hh == 0:
                            S.op("act", lambda a, kc=kc, kk=kk, pf=pf, b=b: a.activation(
                                out=xbv[:, kc, b * 128:(b + 1) * 128], in_=pf[:, kk * 128:(kk + 1) * 128], func=AF.Identity,
                                bias=B2[:, kc:kc + 1], scale=A2[:, kc:kc + 1]), r=[rpf, r_A2, r_B2], w=[rxbs[b * 2 + hh]])
                        else:
                            S.op("dve", lambda v, kc=kc, kk=kk, pf=pf, b=b: v.tensor_scalar(
                                out=xbv[:, kc, b * 128:(b + 1) * 128], in0=pf[:, kk * 128:(kk + 1) * 128],
                                scalar1=A2[:, kc:kc + 1], scalar2=B2[:, kc:kc + 1], op0=ALU.mult, op1=ALU.add),
                                r=[rpf, r_A2, r_B2], w=[rxbs[b * 2 + hh]])
                if blk + 3 < NBLK:
                    gather_block(blk + 3)

        def moe_ffn(e):
            wg_, wu_, wd_, rg, ru, rd = wexp[e % 3]
            xb, rxb_ = xbT[e % 2]
            rxbs = [("xb", e % 2, i_) for i_ in range(4)]
            xbv = xb.rearrange("p (k n) -> p k n", k=KD)
            at, rat_ = actT[e % 2]
            rats = [("actT", e % 2, i_) for i_ in range(4)]
            atv = at.rearrange("p (k n) -> p k n", k=4)
            for f in range(4):
                pg, rpg = half()
                for k in range(KD):
                    S.op("pe", lambda pe, k=k, pg=pg, f=f: pe.matmul(
                        out=pg, lhsT=wg_[:, k, f * 128:(f + 1) * 128], rhs=xbv[:, k, :],
                        start=(k == 0), stop=(k == KD - 1)), r=[rg] + rxbs, w=[rpg])
                pu, rpu = half()
                for k in range(KD):
                    S.op("pe", lambda pe, k=k, pu=pu, f=f: pe.matmul(
                        out=pu, lhsT=wu_[:, k, f * 128:(f + 1) * 128], rhs=xbv[:, k, :],
                        start=(k == 0), stop=(k == KD - 1)), r=[ru] + rxbs, w=[rpu])
                sg_, rsg = sgt[f % 2]
                S.op("act", lambda a, sg_=sg_, pg=pg: a.activation(out=sg_, in_=pg, func=AF.Silu), r=[rpg], w=[rsg])
                S.op("dve", lambda v, sg_=sg_, pu=pu, f=f: v.tensor_tensor(out=atv[:, f, :], in0=pu, in1=sg_, op=ALU.mult),
                     r=[rpu, rsg], w=[rats[f]])
            for b in range(2):
                blk = e * 2 + b
                rsb_, rrs_ = rowsb[blk % 3]
                rrsh = [("rowsb", blk % 3, 0), ("rowsb", blk % 3, 1)]
                for hh in range(2):
                    pf, rpf = full()
                    for f in range(4):
                        S.op("pe", lambda pe, f=f, pf=pf, b=b, hh=hh: pe.matmul(
                            out=pf, lhsT=atv[:, f, b * 128:(b + 1) * 128], rhs=wd_[:, f, hh * 512:(hh + 1) * 512],
                            start=(f == 0), stop=(f == 3)), r=[rd] + rats, w=[rpf])
                    if hh == 0:
                        S.op("act", lambda a, pf=pf, rsb_=rsb_: a.copy(out=rsb_[:, 0:512], in_=pf), r=[rpf], w=[rrsh[0]])
                    else:
                        S.op("dve", lambda v, pf=pf, rsb_=rsb_: v.tensor_copy(out=rsb_[:, 512:1024], in_=pf), r=[rpf], w=[rrsh[1]])
                S.op("sp", lambda q, rsb_=rsb_, blk=blk: q.dma_start(out=rows_dram[blk * 128:(blk + 1) * 128, :], in_=rsb_),
                     r=rrsh, w=[("rows", blk)], key="k_rows%d" % (blk % 3))

        moe_prep(0)
        for e in range(NEXP):
            if e + 2 < NEXP:
                load_expert(e + 2)
            if e + 1 < NEXP:
                moe_prep(e + 1)
            moe_ffn(e)

        S.barrier()
        st["off"] = persist_end
        bc_g2, r_bcg2 = alloc(D)
        bl1g, r_bl1g = alloc(D)
        bl1b, r_bl1b = alloc(D)
        bl2g, r_bl2g = alloc(D)
        bl2b, r_bl2b = alloc(D)
        dgs2 = [alloc(128), alloc(128)]
        S1i, r_S1i = alloc(NSUB, I32)
        S2i, r_S2i = alloc(NSUB, I32)
        stt2, r_stt2 = alloc(12)
        mvt2, r_mvt2 = alloc(2)
        rstd2, r_rstd2 = alloc(1)
        nb2, r_nb2 = alloc(1)
        NBUF = 6
        r1b = [alloc(D) for _ in range(NBUF)]
        r2b = [alloc(D) for _ in range(NBUF)]
        nrb = [alloc(D) for _ in range(NBUF)]
        for (dst, rd, src, kk) in ((bl1g, r_bl1g, ln1g, "k_b1"), (bl1b, r_bl1b, ln1b, "k_b2"),
                                   (bl2g, r_bl2g, ln2g, "k_b3"), (bl2b, r_bl2b, ln2b, "k_b4")):
            S.op("sp", lambda q, dst=dst, src=src: q.dma_start(out=dst, in_=src.partition_broadcast(128)), w=[rd], key=kk)
        S.op("dve", lambda v: v.tensor_copy(out=S1i, in_=S1f), r=[r_S1f], w=[r_S1i])
        S.op("dve", lambda v: v.tensor_copy(out=S2i, in_=S2f), r=[r_S2f], w=[r_S2i])
        S.op("act", lambda a: a.mul(out=bl1g, in_=bl1g, mul=ALPHA), r=[r_bl1g], w=[r_bl1g])
        S.op("act", lambda a: a.mul(out=bl1b, in_=bl1b, mul=ALPHA), r=[r_bl1b], w=[r_bl1b])

        def issue_loads(j):
            r1, rr1 = r1b[j % NBUF]
            r2, rr2 = r2b[j % NBUF]
            nr, rnr = nrb[j % NBUF]
            S.op("sp", lambda q: q.dma_start(out=nr, in_=n_dram[j * 128:(j + 1) * 128, :]), r=[("n_dram",)], w=[rnr],
                 key="k_nr%d" % (j % NBUF))
            S.op("pool", lambda g: g.indirect_dma_start(
                out=r1, out_offset=None, in_=rows_dram, in_offset=bass.IndirectOffsetOnAxis(ap=S1i[:, j:j + 1], axis=0)),
                r=[r_S1i, ("rows",)], w=[rr1], key="k_r1%d" % (j % NBUF))
            S.op("pool", lambda g: g.indirect_dma_start(
                out=r2, out_offset=None, in_=rows_dram, in_offset=bass.IndirectOffsetOnAxis(ap=S2i[:, j:j + 1], axis=0)),
                r=[r_S2i, ("rows",)], w=[rr2], key="k_r2%d" % (j % NBUF))

        issue_loads(0)
        issue_loads(1)
        issue_loads(2)
        make_bcast(bc_g2, r_bcg2, g2p, r_g2p, dgs2)

        def stage1(j):
            r1, rr1 = r1b[j % NBUF]
            r2, rr2 = r2b[j % NBUF]
            nr, rnr = nrb[j % NBUF]
            S.op("pool", lambda g: g.tensor_tensor(out=nr, in0=nr, in1=bl1g, op=ALU.mult), r=[rnr, r_bl1g], w=[rnr])
            S.op("pool", lambda g: g.tensor_tensor(out=nr, in0=nr, in1=bl1b, op=ALU.add), r=[rnr, r_bl1b], w=[rnr])
            S.op("act", lambda a: a.activation(out=r1, in_=r1, func=AF.Identity, scale=W1[:, j:j + 1]),
                 r=[rr1, r_W1], w=[rr1])
            S.op("dve", lambda v: v.scalar_tensor_tensor(
                out=r2, in0=r2, scalar=W2[:, j:j + 1], in1=r1, op0=ALU.mult, op1=ALU.add), r=[rr2, rr1, r_W2], w=[rr2])
            S.op("dve", lambda v: v.tensor_tensor(out=r2, in0=r2, in1=bc_g2, op=ALU.mult), r=[rr2, r_bcg2], w=[rr2])
            S.op("pool", lambda g: g.tensor_tensor(out=r2, in0=r2, in1=nr, op=ALU.add), r=[rnr, rr2], w=[rr2])

        def stage2(j):
            r1, rr1 = r1b[j % NBUF]
            r2, rr2 = r2b[j % NBUF]
            layer_norm_stats(r2, rr2, stt2, r_stt2, mvt2, r_mvt2, rstd2, r_rstd2, nb2, r_nb2)
            S.op("act", lambda a: a.activation(out=r1, in_=r2, func=AF.Identity, bias=nb2[:, 0:1], scale=rstd2[:, 0:1]),
                 r=[rr2, r_nb2, r_rstd2], w=[rr1])
            S.op("dve", lambda v: v.tensor_tensor(out=r1, in0=r1, in1=bl2g, op=ALU.mult), r=[rr1, r_bl2g], w=[rr1])
            S.op("dve", lambda v: v.tensor_tensor(out=r1, in0=r1, in1=bl2b, op=ALU.add), r=[rr1, r_bl2b], w=[rr1])
            S.op("sp", lambda q: q.dma_start(out=out[j * 128:(j + 1) * 128, :], in_=r1), r=[rr1], w=[("out", j)],
                 key="k_out%d" % (j % NBUF))

        stage1(0)
        for j in range(NSUB):
            if j + 3 < NSUB:
                issue_loads(j + 3)
            if j + 1 < NSUB:
                stage1(j + 1)
            stage2(j)

        keys = sorted(S.dma_cnt.keys())
        S.number()
        with contextlib.ExitStack() as es:
            engsem = {e: es.enter_context(nc.semaphore("s_" + e)) for e in ("pe", "act", "dve", "pool")}
            dmasem = {k: es.enter_context(nc.semaphore(k)) for k in keys}
            block = es.enter_context(nc.Block())

            @block.tensor
            def _(pe):
                S.emit_engine("pe", pe, engsem, dmasem)

            @block.scalar
            def _(a):
                S.emit_engine("act", a, engsem, dmasem)

            @block.vector
            def _(v):
                S.emit_engine("dve", v, engsem, dmasem)

            @block.gpsimd
            def _(g):
                S.emit_engine("pool", g, engsem, dmasem)

            @block.sync
            def _(q):
                S.emit_engine("sp", q, engsem, dmasem)
    print("ops:", S.n, {e: len(S.ops[e]) for e in ENGS}, "dma keys:", len(keys))
    return nc


_CACHE = {}


def _prep_inputs(inputs):
    f = lambda a: np.ascontiguousarray(np.asarray(a, dtype=np.float32))
    x = f(inputs["x"])
    c = f(inputs["c"])
    col = lambda v: np.ascontiguousarray(f(v).reshape(-1, 128).T)
    shared = {
        "w_ada": f(inputs["w_ada"][0]),
        "b_adaT": col(inputs["b_ada"][0]),
        "w_in": f(inputs["w_in"][0]),
        "cwT": np.ascontiguousarray(f(inputs["conv_w"][0]).reshape(3, KD, 128).transpose(2, 1, 0).reshape(128, KD * 3)),
        "w_oc": f(inputs["w_out_conv"][0]),
        "w_pool": f(inputs["w_pool"][0]),
        "pscT": col(inputs["pool_scale"][0]),
        "w_o": f(inputs["w_o"][0]),
        "ln1gT": col(inputs["ln1_g"][0]),
        "ln1bT": col(inputs["ln1_b"][0]),
        "ln1g": f(inputs["ln1_g"][0]),
        "ln1b": f(inputs["ln1_b"][0]),
        "ln2g": f(inputs["ln2_g"][0]),
        "ln2b": f(inputs["ln2_b"][0]),
        "w_rt": np.ascontiguousarray(np.concatenate([f(inputs["w_group"][0]), f(inputs["w_router"][0])], axis=1)),
        "b_rt": np.ascontiguousarray(np.concatenate([f(inputs["b_group"][0]).reshape(-1), f(inputs["b_router"][0]).reshape(-1)])),
        "w_gate": f(inputs["w_gate"][0]),
        "w_up": f(inputs["w_up"][0]),
        "w_down": f(inputs["w_down"][0]),
    }
    in_maps = []
    for r in range(NCORES):
        b, hf = r // 2, r % 2
        t0 = hf * TOK
        halo = x[b, t0 - HL:t0] if hf else np.zeros((HL, D), np.float32)
        tt = np.arange(T) + t0 + 1
        ic = np.concatenate([1.0 / np.minimum(tt, w) for w in WINS]).astype(np.float32)
        m = dict(shared)
        m["xs"] = np.ascontiguousarray(np.concatenate([halo, x[b, t0:t0 + TOK]], axis=0))
        m["cvec"] = col(c[b])
        m["hm"] = np.full((128, 1), float(hf), np.float32)
        m["icnt"] = np.ascontiguousarray(np.broadcast_to(ic[None, :], (128, 4 * T)))
        in_maps.append(m)
    return in_maps


def kernel(**inputs):
    if "nc" not in _CACHE:
        _CACHE["nc"] = build_program()
    nc = _CACHE["nc"]
    in_maps = _prep_inputs(inputs)
    res = run_bass_kernel_spmd(nc, in_maps, core_ids=list(range(NCORES)))
    outs = [np.asarray(res.results[r]["out"], dtype=np.float32) for r in range(NCORES)]
    return np.concatenate(outs, axis=0).reshape(4, SEQ, D)
```
